# Optimizing a Trainium2 kernel written in Bass

```python
import jax, jax.numpy as jnp
from jax import lax
import numpy as np

D_MODEL = 2048
BATCH = 4
SEQ = 2048
DEPTH = 4
DEC_BATCH = 32
DEC_SEQ = 1
PAST_LEN = 16384
PAGE_SIZE = 128

BRANCH_WIDTH = D_MODEL // 2
N_BRANCH = 3
GLA_HEADS = 4
GLA_DV = BRANCH_WIDTH // GLA_HEADS
GLA_DK = GLA_DV // 2
GLA_RANK = 16
GLA_TAU = 16.0
GLA_CHUNK = 64
CONV_WIDTH = BRANCH_WIDTH
CONV_K = 3
SWA_HD = 64
SWA_HEADS = BRANCH_WIDTH // SWA_HD
SWA_KV_HEADS = SWA_HEADS // 4
SWA_GROUP = SWA_HEADS // SWA_KV_HEADS
WINDOW = 128
D_FF = 4 * D_MODEL
EPS = 1e-6

SPLIT_SIZES = (
    GLA_HEADS * GLA_DK,
    GLA_HEADS * GLA_DK,
    GLA_HEADS * GLA_DV,
    GLA_HEADS * GLA_DV,
    GLA_RANK,
    CONV_WIDTH,
    CONV_WIDTH,
    CONV_WIDTH,
    SWA_HEADS * SWA_HD,
    SWA_KV_HEADS * SWA_HD,
    SWA_KV_HEADS * SWA_HD,
    N_BRANCH * D_MODEL,
)
IN_COLS = sum(SPLIT_SIZES)

kernel_name = 'hybrid_gla_shortconv_swa_gated_decode_step'


def _split_points():
    pts, acc = [], 0
    for s in SPLIT_SIZES[:-1]:
        acc += s
        pts.append(acc)
    return pts


def _rmsnorm(x, g):
    xf = x.astype(jnp.float32)
    y = xf * lax.rsqrt(jnp.mean(xf * xf, axis=-1, keepdims=True) + EPS)
    return (y * g.astype(jnp.float32)).astype(x.dtype)


def _gla(q, k, v, log_a, s0):
    B, L = q.shape[:2]
    C = min(GLA_CHUNK, L)
    n = -(-L // C)
    pad = n * C - L
    if pad:
        padf = lambda t: jnp.pad(t, ((0, 0), (0, pad), (0, 0), (0, 0)))
        q, k, v, log_a = padf(q), padf(k), padf(v), padf(log_a)
    rs = lambda t: t.reshape(B, n, C, GLA_HEADS, t.shape[-1]).transpose(0, 3, 1, 2, 4)
    q, k, v, log_a = rs(q), rs(k), rs(v), rs(log_a)
    b = jnp.cumsum(log_a, axis=3)
    b_last = b[:, :, :, -1:, :]
    qt = q * jnp.exp(b)
    kt = k * jnp.exp(-b)
    kd = k * jnp.exp(b_last - b)
    causal = jnp.tril(jnp.ones((C, C), dtype=bool))
    A = jnp.where(causal, jnp.einsum('bhnck,bhnsk->bhncs', qt, kt), 0.0)
    o_intra = jnp.einsum('bhncs,bhnsv->bhncv', A, v)
    U = jnp.einsum('bhnck,bhncv->bhnkv', kd, v)
    g = jnp.exp(b_last[:, :, :, 0, :])

    def step(S, inp):
        g_n, U_n = inp
        return g_n[..., None] * S + U_n, S

    S_fin, S_in = lax.scan(step, s0, (jnp.moveaxis(g, 2, 0), jnp.moveaxis(U, 2, 0)))
    S_in = jnp.moveaxis(S_in, 0, 2)
    o = jnp.einsum('bhnck,bhnkv->bhncv', qt, S_in) + o_intra
    o = o.transpose(0, 2, 3, 1, 4).reshape(B, n * C, GLA_HEADS, GLA_DV)[:, :L]
    return o, S_fin


def _sink_alibi_attn(q, k, v, q_pos, k_pos, sinks):
    slopes = jnp.exp2(-8.0 * jnp.arange(1, SWA_HEADS + 1, dtype=jnp.float32) / SWA_HEADS)
    slopes = slopes.reshape(SWA_KV_HEADS, SWA_GROUP)[:, :, None, None]
    s = jnp.einsum('...tkgd,...skd->...kgts', q, k).astype(jnp.float32) * (SWA_HD ** -0.5)
    dist = q_pos[..., :, None] - k_pos[..., None, :]
    valid = (dist >= 0) & (dist < WINDOW) & (k_pos[..., None, :] >= 0)
    dist = dist[..., None, None, :, :].astype(jnp.float32)
    valid = valid[..., None, None, :, :]
    s = jnp.where(valid, s - slopes * dist, -jnp.inf)
    sink = sinks.astype(jnp.float32).reshape(SWA_KV_HEADS, SWA_GROUP)[:, :, None, None]
    m = jnp.maximum(jnp.max(s, axis=-1, keepdims=True), sink)
    p = jnp.exp(s - m)
    p = p / (jnp.sum(p, axis=-1, keepdims=True) + jnp.exp(sink - m))
    return jnp.einsum('...kgts,...skd->...tkgd', p.astype(v.dtype), v)


def _swa_prompt(q, k, v, sinks):
    B, L = q.shape[:2]
    nb = L // WINDOW
    qb = q.reshape(B, nb, WINDOW, SWA_KV_HEADS, SWA_GROUP, SWA_HD)

    def band(t):
        tp = jnp.pad(t, ((0, 0), (WINDOW, 0), (0, 0), (0, 0)))
        tp = tp.reshape(B, nb + 1, WINDOW, SWA_KV_HEADS, SWA_HD)
        return jnp.concatenate([tp[:, :-1], tp[:, 1:]], axis=2)

    pos = jnp.arange(L, dtype=jnp.int32).reshape(nb, WINDOW)
    k_pos = jnp.concatenate([pos - WINDOW, pos], axis=1)
    o = _sink_alibi_attn(qb, band(k), band(v), pos, k_pos, sinks)
    return o.reshape(B, L, SWA_HEADS * SWA_HD), k[:, -WINDOW:], v[:, -WINDOW:]


def _swa_sample(q, k, v, kc, vc, sinks):
    B, T = q.shape[:2]
    kk = jnp.concatenate([kc.astype(k.dtype), k], axis=1)
    vv = jnp.concatenate([vc.astype(v.dtype), v], axis=1)
    k_pos = PAST_LEN - WINDOW + jnp.arange(WINDOW + T, dtype=jnp.int32)
    q_pos = PAST_LEN + jnp.arange(T, dtype=jnp.int32)
    o = _sink_alibi_attn(q, kk, vv, q_pos, k_pos, sinks)
    return o.reshape(B, T, SWA_HEADS * SWA_HD), kk[:, -WINDOW:], vv[:, -WINDOW:]


def _mixer(h, w_in, w_lr, b_lr, gla_norm, conv_w, sinks, w_branch, w_out,
           gla_s0, conv_prev, kc, vc):
    B, L, _ = h.shape
    f32 = jnp.float32
    (gq, gk, gv, gr, glr, cb, cc, ch, sq, sk, sv, gates) = jnp.split(
        h @ w_in, _split_points(), axis=-1)
    log_a = jax.nn.log_sigmoid((glr @ w_lr + b_lr).astype(f32)) / GLA_TAU
    log_a = log_a.reshape(B, L, GLA_HEADS, GLA_DK)
    q = gq.reshape(B, L, GLA_HEADS, GLA_DK).astype(f32) * (GLA_DK ** -0.5)
    k = gk.reshape(B, L, GLA_HEADS, GLA_DK).astype(f32)
    v = gv.reshape(B, L, GLA_HEADS, GLA_DV).astype(f32)
    o, gla_state = _gla(q, k, v, log_a, gla_s0.astype(f32))
    o = _rmsnorm(o, gla_norm).reshape(B, L, GLA_HEADS * GLA_DV).astype(h.dtype)
    out_a = o * jax.nn.silu(gr)
    u = cc * ch
    cat = jnp.concatenate([conv_prev.astype(u.dtype), u], axis=1)
    conv = conv_w[0] * cat[:, 0:L] + conv_w[1] * cat[:, 1:L + 1] + conv_w[2] * cat[:, 2:L + 2]
    out_b = cb * conv
    conv_state = cat[:, -(CONV_K - 1):]
    qs = sq.reshape(B, L, SWA_KV_HEADS, SWA_GROUP, SWA_HD)
    ks = sk.reshape(B, L, SWA_KV_HEADS, SWA_HD)
    vs = sv.reshape(B, L, SWA_KV_HEADS, SWA_HD)
    if kc is None:
        out_c, k_buf, v_buf = _swa_prompt(qs, ks, vs, sinks)
    else:
        out_c, k_buf, v_buf = _swa_sample(qs, ks, vs, kc, vc, sinks)
    br = jnp.stack([out_a, out_b, out_c], axis=2)
    br = jnp.einsum('blnc,ncd->blnd', br, w_branch)
    g = jax.nn.sigmoid(gates.reshape(B, L, N_BRANCH, D_MODEL))
    y = jnp.sum(g * br, axis=2) @ w_out
    return y, gla_state, conv_state, k_buf, v_buf


def _mlp(h, w_up, w_down):
    return jnp.square(jax.nn.relu(h @ w_up)) @ w_down


def setup_inputs(seed: int = 0) -> dict:
    key = jax.random.key(seed)
    ks = jax.random.split(key, 20)
    nrm = lambda k, shape, scale: jax.random.normal(k, shape, jnp.float32) * scale
    return {
        'x_prompt': nrm(ks[0], (BATCH, SEQ, D_MODEL), 1.0),
        'x_sample': nrm(ks[1], (DEC_BATCH, DEC_SEQ, D_MODEL), 1.0),
        'state_gla': nrm(ks[2], (DEPTH, DEC_BATCH, GLA_HEADS, GLA_DK, GLA_DV), 1.0),
        'state_conv': nrm(ks[3], (DEPTH, DEC_BATCH, CONV_K - 1, CONV_WIDTH), 1.0),
        'cache_k': nrm(ks[4], (DEPTH, DEC_BATCH, WINDOW, SWA_KV_HEADS, SWA_HD), 1.0),
        'cache_v': nrm(ks[5], (DEPTH, DEC_BATCH, WINDOW, SWA_KV_HEADS, SWA_HD), 1.0),
        'w_in': nrm(ks[6], (DEPTH, D_MODEL, IN_COLS), D_MODEL ** -0.5),
        'w_lr': nrm(ks[7], (DEPTH, GLA_RANK, GLA_HEADS * GLA_DK), GLA_RANK ** -0.5),
        'b_lr': nrm(ks[8], (DEPTH, GLA_HEADS * GLA_DK), 0.1),
        'gla_norm': 1.0 + nrm(ks[9], (DEPTH, GLA_DV), 0.01),
        'conv_w': nrm(ks[10], (DEPTH, CONV_K, CONV_WIDTH), CONV_K ** -0.5),
        'attn_sinks': nrm(ks[11], (DEPTH, SWA_HEADS), 1.0),
        'w_branch': nrm(ks[12], (DEPTH, N_BRANCH, BRANCH_WIDTH, D_MODEL), BRANCH_WIDTH ** -0.5),
        'w_out': nrm(ks[13], (DEPTH, D_MODEL, D_MODEL), D_MODEL ** -0.5),
        'norm_mix': 1.0 + nrm(ks[14], (DEPTH, D_MODEL), 0.01),
        'norm_mlp': 1.0 + nrm(ks[15], (DEPTH, D_MODEL), 0.01),
        'w_up': nrm(ks[16], (DEPTH, D_MODEL, D_FF), D_MODEL ** -0.5),
        'w_down': nrm(ks[17], (DEPTH, D_FF, D_MODEL), D_FF ** -0.5),
        'norm_final': 1.0 + nrm(ks[18], (D_MODEL,), 0.01),
    }


def reference(x_prompt, x_sample, state_gla, state_conv, cache_k, cache_v,
              w_in, w_lr, b_lr, gla_norm, conv_w, attn_sinks, w_branch, w_out,
              norm_mix, norm_mlp, w_up, w_down, norm_final):
    xp, xs = x_prompt, x_sample
    Bp = xp.shape[0]
    sg_p, sg_s, sc_p, sc_s, kp_l, ks_l, vp_l, vs_l = [], [], [], [], [], [], [], []
    for l in range(DEPTH):
        lw = (w_in[l], w_lr[l], b_lr[l], gla_norm[l], conv_w[l], attn_sinks[l],
              w_branch[l], w_out[l])
        s0 = jnp.zeros((Bp, GLA_HEADS, GLA_DK, GLA_DV), jnp.float32)
        c0 = jnp.zeros((Bp, CONV_K - 1, CONV_WIDTH), xp.dtype)
        yp, gp, cp, kp, vp = _mixer(_rmsnorm(xp, norm_mix[l]), *lw, s0, c0, None, None)
        ys, gs, cs, kss, vss = _mixer(_rmsnorm(xs, norm_mix[l]), *lw, state_gla[l],
                                      state_conv[l], cache_k[l], cache_v[l])
        xp = xp + yp
        xs = xs + ys
        xp = xp + _mlp(_rmsnorm(xp, norm_mlp[l]), w_up[l], w_down[l])
        xs = xs + _mlp(_rmsnorm(xs, norm_mlp[l]), w_up[l], w_down[l])
        sg_p.append(gp); sg_s.append(gs); sc_p.append(cp); sc_s.append(cs)
        kp_l.append(kp); ks_l.append(kss); vp_l.append(vp); vs_l.append(vss)
    y_prompt = _rmsnorm(xp, norm_final)
    y_sample = _rmsnorm(xs, norm_final)
    state_gla_prompt = jnp.stack(sg_p)
    state_gla_sample = jnp.stack(sg_s)
    state_conv_prompt = jnp.stack(sc_p)
    state_conv_sample = jnp.stack(sc_s)
    cache_k_prompt = jnp.stack(kp_l)
    cache_k_sample = jnp.stack(ks_l)
    cache_v_prompt = jnp.stack(vp_l)
    cache_v_sample = jnp.stack(vs_l)
    return (y_prompt, y_sample, state_gla_prompt, state_gla_sample,
            state_conv_prompt, state_conv_sample, cache_k_prompt, cache_k_sample,
            cache_v_prompt, cache_v_sample)
```

```python
import math
import numpy as np
import concourse.bass as bass
import concourse.mybir as mybir
from concourse.bass_utils import run_bass_kernel_spmd

F32 = mybir.dt.float32
BF16 = mybir.dt.bfloat16
AF = mybir.ActivationFunctionType
ALU = mybir.AluOpType
AX = mybir.AxisListType

PAGE = 512
SEM_LIM = 20000
NDMA = 8
SB_BASE = 16896
SB_END = 229344

D = 2048
DEPTH = 4
SEQ = 2048
TT = 512
NSMP = 4
C_Q, C_K, C_V, C_GR, C_GLR, C_CB, C_CC, C_CH, C_SQ, C_SK, C_SV, C_G, C_END = (
    0, 512, 1024, 2048, 3072, 3088, 4112, 5136, 6160, 7184, 7440, 7696, 13840)
EPS = 1e-6
SLOPES = [2.0 ** (-8.0 * (i + 1) / 16) for i in range(16)]
P_NMIX, P_NMLP, P_BLR, P_GN, P_CW, P_SINK, P_SINKS, P_BLRBC, P_NFIN, NPRM = 0, 16, 32, 36, 38, 62, 78, 82, 594, 610
K_ONES, K_ID, K_TRI, K_GT, K_BD, K_DP, K_DPF, K_SEL, K_BS, NCST = 0, 128, 256, 384, 512, 640, 896, 1152, 1664, 2176


import os
DBG_NOSAMPLE = bool(int(os.environ.get("K_NOSAMPLE", "0")))
DBG_STOP = int(os.environ.get("K_STOP", "99"))
DBG_SSTOP = int(os.environ.get("K_SSTOP", "99"))
DBG_SWS = int(os.environ.get("K_SWS", "99"))
DBG_M = int(os.environ.get("K_M", "255"))


class Acc:
    __slots__ = ("ap", "keys")

    def __init__(self, ap, keys):
        self.ap = ap
        self.keys = keys


class Buf:
    def __init__(self, nc, name, shape, dtype, offset):
        self.t = nc.alloc_sbuf_tensor_at(name, list(shape), dtype, offset=offset)
        self.base = offset
        self.esz = 4 if dtype == F32 else 2
        self.shape = list(shape)
        st = [1] * len(shape)
        for d in range(len(shape) - 2, 0, -1):
            st[d] = st[d + 1] * shape[d + 1]
        self.st = st
        n = 1
        for s in shape[1:]:
            n *= s
        self.nbytes = n * self.esz
        assert offset >= SB_BASE and offset + self.nbytes <= SB_END, (name, offset, self.nbytes)

    def keys_for(self, idx):
        if not isinstance(idx, tuple):
            idx = (idx,)
        lo = 0
        hi = 0
        for d in range(1, len(self.shape)):
            i = idx[d] if d < len(idx) else slice(None)
            if isinstance(i, slice):
                a = 0 if i.start is None else i.start
                b = self.shape[d] if i.stop is None else i.stop
                lo += a * self.st[d]
                hi += (b - 1) * self.st[d]
            else:
                lo += i * self.st[d]
                hi += i * self.st[d]
        lob = self.base + lo * self.esz
        hib = self.base + (hi + 1) * self.esz
        return range(lob // PAGE, (hib - 1) // PAGE + 1)

    def __getitem__(self, idx):
        return Acc(self.t[idx], self.keys_for(idx))

    def acc(self, ap, idx):
        return Acc(ap, self.keys_for(idx))


class PBuf:
    def __init__(self, nc, name, bank, dtype=F32):
        self.ncol = 512 if dtype == F32 else 1024
        self.t = nc.alloc_psum_tensor(name, [128, self.ncol], dtype)
        self.bank = bank
        self.q = self.ncol // 4

    def __getitem__(self, idx):
        cs = idx[1]
        a = 0 if cs.start is None else cs.start
        b = self.ncol if cs.stop is None else cs.stop
        keys = (("ps", self.bank),)
        return Acc(self.t[idx], keys)

    def acc(self, ap, a, b):
        keys = (("ps", self.bank),)
        return Acc(ap, keys)


class Sched:
    ENGS = ("pe", "act", "dve", "pool", "sp")

    def __init__(self, nc):
        self.nc = nc
        self.q = {e: [] for e in self.ENGS}
        self.semh = {}
        self.seen = {e: {} for e in self.ENGS}
        self.state = {}
        self.dma_val = {q: [0] * NDMA for q in ("sp", "pool")}
        self.dma_rr = {"sp": 0, "pool": 0}
        self.n_ops = 0
        for qn in ("sp", "pool"):
            for i in range(NDMA):
                self.semh[("dma" + qn, i)] = nc.alloc_semaphore(name=f"sd{qn}{i}")

    def _sem(self, sk):
        h = self.semh.get(sk)
        if h is None:
            h = self.nc.alloc_semaphore(name=f"s_{sk[0]}_{sk[1]}")
            self.semh[sk] = h
        return h

    def _deps(self, eng, reads, writes):
        deps = {}
        st = self.state
        for a in reads:
            for k in a.keys:
                s = st.get(k)
                if s is not None and s[0] is not None:
                    sk, v = s[0]
                    if v > deps.get(sk, -1):
                        deps[sk] = v
        for a in writes:
            for k in a.keys:
                s = st.get(k)
                if s is not None:
                    if s[0] is not None:
                        sk, v = s[0]
                        if v > deps.get(sk, -1):
                            deps[sk] = v
                    for sk, v in s[1].items():
                        if v > deps.get(sk, -1):
                            deps[sk] = v
        waits = []
        seen = self.seen[eng]
        for sk, v in deps.items():
            if eng == "pe" and sk == "pe":
                continue
            if seen.get(sk, -1) >= v:
                continue
            seen[sk] = v
            if isinstance(sk, str):
                self.q[sk][v][2] = True
            waits.append((sk, v))
        return waits

    def _record(self, reads, writes, sk, v):
        st = self.state
        for a in reads:
            for k in a.keys:
                s = st.get(k)
                if s is None:
                    st[k] = [None, {sk: v}]
                elif v > s[1].get(sk, -1):
                    s[1][sk] = v
        for a in writes:
            for k in a.keys:
                st[k] = [(sk, v), {}]

    def op(self, eng, fn, reads=(), writes=(), inc=True):
        waits = self._deps(eng, reads, writes)
        idx = len(self.q[eng])
        self.q[eng].append([waits, fn, False, None])
        self._record(reads, writes, eng, idx)
        self.n_ops += 1

    def dma(self, qeng, out, in_, reads=(), writes=(), **kw):
        i = self.dma_rr[qeng]
        self.dma_rr[qeng] = (i + 1) % NDMA
        sk = ("dma" + qeng, i)
        waits = self._deps(qeng, reads, writes)
        prev = self.dma_val[qeng][i]
        if prev > 0 and self.seen[qeng].get(sk, -1) < prev:
            self.seen[qeng][sk] = prev
            waits.append((sk, prev))
        v = prev + 16
        self.dma_val[qeng][i] = v

        def fn(e, out=out, in_=in_, kw=kw):
            return e.dma_start(out=out, in_=in_, **kw)

        self.q[qeng].append([waits, fn, False, sk])
        self._record(reads, writes, sk, v)
        self.n_ops += 1

    def emit(self):
        nc = self.nc
        val = {}
        for eng in self.ENGS:
            ep, cnt = 0, 0
            for idx, ent in enumerate(self.q[eng]):
                if ent[2]:
                    if cnt >= SEM_LIM:
                        ep += 1
                        cnt = 0
                    cnt += 1
                    val[(eng, idx)] = (self._sem((eng, ep)), cnt)
        fin = []
        for qn in ("sp", "pool"):
            for i in range(NDMA):
                if self.dma_val[qn][i] > 0:
                    fin.append((self._sem(("dma" + qn, i)), self.dma_val[qn][i]))
        q = self.q
        self.n_marked = len(val)

        def replay(eng, e):
            for idx, (waits, fn, marked, dsem) in enumerate(q[eng]):
                for (sk, v) in waits:
                    if isinstance(sk, str):
                        h, vv = val[(sk, v)]
                    else:
                        h, vv = self._sem(sk), v
                    e.wait_ge(h, vv)
                ins = fn(e)
                if dsem is not None:
                    ins.then_inc(self._sem(dsem), 16)
                elif marked:
                    ins.then_inc(val[(eng, idx)][0], 1)

        with nc.Block() as block:
            @block.tensor
            def _(e):
                replay("pe", e)

            @block.scalar
            def _(e):
                replay("act", e)

            @block.vector
            def _(e):
                replay("dve", e)

            @block.gpsimd
            def _(e):
                replay("pool", e)

            @block.sync
            def _(e):
                replay("sp", e)
                for (h, v) in fin:
                    e.wait_ge(h, v)


def _isacc(x):
    return isinstance(x, Acc)


class Kern:
    def __init__(self, nc, depth, ntile, wseq=None):
        self.nc = nc
        self.depth = depth
        self.ntile = ntile
        self.S = Sched(nc)
        self.wseq = wseq
        self.wrec = []
        self.widx = 0
        self.wissued = 0
        self.evi = 0
        self._dram()
        self._sbuf()

    def _dram(self):
        nc, L, NTI = self.nc, self.depth, self.ntile
        di = lambda n, s: nc.dram_tensor(n, s, F32, kind="ExternalInput").ap()
        do = lambda n, s: nc.dram_tensor(n, s, F32, kind="ExternalOutput").ap()
        self.d_xp = di("xp", [NTI, 128, 16, TT])
        self.d_xs = di("xs", [128, 16, NSMP])
        self.d_sg = di("sg", [L, NSMP, 4, 128, 256])
        self.d_sc = di("sc", [L, 128, 8, NSMP, 2])
        self.d_ck = di("ck", [L, NSMP, 128, 256])
        self.d_ckT = di("ckT", [L, 64, NSMP, 4, 128])
        self.d_cv = di("cv", [L, NSMP, 128, 256])
        self.d_win = di("w_in", [L, D, C_END])
        self.d_wlr = di("w_lr", [L, 16, 512])
        self.d_wbr = di("w_branch", [L, 3, 1024, D])
        self.d_wout = di("w_out", [L, D, D])
        self.d_wup = di("w_up", [L, D, 4 * D])
        self.d_wdn = di("w_down", [L, 4 * D, D])
        self.d_prm = di("prm", [L, 128, NPRM])
        self.d_cst = di("cst", [128, NCST])
        self.o_yp = do("o_yp", [NTI, 128, 16, TT])
        self.o_ys = do("o_ys", [128, 16, NSMP])
        self.o_sgp = do("o_sgp", [L, 4, 128, 256])
        self.o_sgs = do("o_sgs", [L, NSMP, 4, 128, 256])
        self.o_scp = do("o_scp", [L, 128, 8, 2])
        self.o_scs = do("o_scs", [L, 128, 8, NSMP, 2])
        self.o_ckp = do("o_ckp", [L, 128, 256])
        self.o_cks = do("o_cks", [L, NSMP, 128, 256])
        self.o_cvp = do("o_cvp", [L, 128, 256])
        self.o_cvs = do("o_cvs", [L, NSMP, 128, 256])

    def _sbuf(self):
        nc = self.nc
        off = [SB_BASE]

        def pa(name, shape, dt):
            b = Buf(nc, name, shape, dt, off[0])
            off[0] += (b.nbytes + PAGE - 1) // PAGE * PAGE
            return b

        L = self.depth
        self.cst = pa("cst", [128, NCST], F32)
        self.identb = pa("identb", [128, 128], BF16)
        self.cols = pa("cols", [128, 8], F32)
        self.xT = pa("xT", [128, 16, TT], F32)
        self.hT = pa("hT", [128, 16, TT], BF16)
        self.prm2 = [pa(f"prm{i}", [128, NPRM], F32) for i in range(2)]
        self.wlr = pa("wlr", [16, 512], F32)
        self.Sst = [pa(f"S{l}", [128, 4, 256], F32) for l in range(L)]
        self.uprev = [pa(f"up{l}", [128, 8, 2], F32) for l in range(L)]
        self.kprev = [pa(f"kp{l}", [128, 4, 128], BF16) for l in range(L)]
        self.vprev = [pa(f"vp{l}", [128, 512], BF16) for l in range(L)]
        self.wslot = [pa(f"ws{i}", [128, 16, 256], BF16) for i in range(3)]
        self.xs = pa("xs", [128, 16, NSMP], F32)
        self.hTs = pa("hTs", [128, 16, NSMP], BF16)
        self.oas = pa("oas", [128, 8, NSMP], BF16)
        self.obs = pa("obs", [128, 8, NSMP], BF16)
        self.ocs = pa("ocs", [128, 8, NSMP], BF16)
        self.ypres = pa("ypres", [128, 16, NSMP], BF16)
        self.hids = pa("hids", [128, 64, NSMP], BF16)
        self.smisc = pa("smisc", [128, 256], F32)
        A = off[0]
        self.A = A
        assert A + 86016 <= SB_END, (A, SB_END - A)

        def ar(name, shape, dt, o):
            return Buf(nc, name, shape, dt, A + o)

        self.out_a = ar("out_a", [128, 8, TT], BF16, 0)
        self.out_b = ar("out_b", [128, 8, TT], BF16, 8192)
        self.out_c = ar("out_c", [128, 8, TT], BF16, 16384)
        self.gr_s = ar("gr_s", [128, 8, TT], BF16, 24576)
        self.v_tok = ar("v_tok", [128, 4, 1024], BF16, 32768)
        self.qT = ar("qT", [128, 4, TT], BF16, 40960)
        self.kT = ar("kT", [128, 4, TT], BF16, 45056)
        self.kd = ar("kd", [128, 4, 512], BF16, 49152)
        self.sp_tok = ar("sp_tok", [128, 4, 512], F32, 53248)
        self.expnb = ar("expnb", [128, 4, TT], F32, 61440)
        self.expb = ar("expb", [128, 4, TT], F32, 69632)
        self.ekd = ar("ekd", [128, 4, 512], F32, 77824)
        self.oT = ar("oT", [128, 8, TT], F32, 53248)
        self.sqt = [ar(f"sqt{i}", [128, TT], F32, 77824 + 2048 * i) for i in range(2)]
        self.rs = ar("rs", [128, TT], F32, 81920)
        self.tmpf = ar("tmpf", [128, TT], F32, 83968)
        self.ATm = [ar(f"ATm{i}", [128, 128], BF16, 8192 + 256 * i) for i in range(4)]
        self.Sbf = ar("Sbf", [128, 4, 256], BF16, 9216)
        self.glr = ar("glr", [16, 512], F32, 11264)
        self.gcol = ar("gcol", [128, 4, 8], F32, 13312)
        self.ccs = ar("ccs", [128, 2, TT], F32, 32768)
        self.ubuf = ar("ubuf", [128, 2, TT + 2], F32, 36864)
        self.ct = [ar(f"ct{i}", [128, TT], F32, 41472 + 2048 * i) for i in range(2)]
        self.qsT = ar("qsT", [128, 8, TT], BF16, 24576)
        self.kdupT = ar("kdupT", [128, 4, 640], BF16, 32768)
        self.vdup = ar("vdup", [128, 5, 512], BF16, 37888)
        self.sb = [ar(f"sb{i}", [128, 256], F32, 43008 + 1024 * i) for i in range(2)]
        self.pex = [ar(f"pex{i}", [128, 256], F32, 45056 + 1024 * i) for i in range(2)]
        self.pn = [ar(f"pn{i}", [128, 256], BF16, 47104 + 512 * i) for i in range(4)]
        self.pTsb = [ar(f"pTsb{i}", [128, 256], BF16, 49152 + 512 * i) for i in range(4)]
        self.sc8 = [ar(f"sc8{i}", [128, 8], F32, 51200 + 512 * i) for i in range(4)]
        self.kout = ar("kout", [128, 256], F32, 43008)
        self.vout = ar("vout", [128, 256], F32, 44032)
        self.sig = [ar(f"sig{i}", [128, TT], F32, 24576 + 2048 * i) for i in range(2)]
        self.accm = [ar(f"accm{i}", [128, TT], F32, 28672 + 2048 * i) for i in range(2)]
        self.tmul = [ar(f"tmul{i}", [128, TT], F32, 32768 + 2048 * i) for i in range(2)]
        self.ypre = ar("ypre", [128, 16, TT], BF16, 65536)
        self.sigB = [ar(f"sigB{i}", [128, TT], F32, 53248 + 2048 * i) for i in range(2)]
        self.accmB = [ar(f"accmB{i}", [128, TT], F32, 57344 + 2048 * i) for i in range(2)]
        self.tmulB = [ar(f"tmulB{i}", [128, TT], F32, 61440 + 2048 * i) for i in range(2)]
        self.mcnt = 0
        self.hid = ar("hid", [128, 64, TT], BF16, 0)
        self.relu = [ar(f"relu{i}", [128, TT], F32, 65536 + 2048 * i) for i in range(2)]
        self.yout = [ar(f"yout{i}", [128, TT], F32, 69632 + 2048 * i) for i in range(2)]
        self.v_tok_s = ar("v_tok_s", [4, 1024], F32, 16384)
        self.stg = [ar(f"stg{i}", [128, 256], F32, 20480 + 1024 * i) for i in range(2)]
        self.gsm = ar("gsm", [128, 256], F32, 22528)
        self.grss = ar("grss", [128, 8, NSMP], BF16, 23552)
        self.csm = ar("csm", [128, 512], F32, 16384)
        self.kwinT = ar("kwinT", [64, NSMP, 4, 128], BF16, 53248)
        self.vwin = ar("vwin", [128, NSMP, 512], BF16, 57344)
        self.k_tok_s = ar("k_tok_s", [4, 256], F32, 61440)
        self.v_tok_s2 = ar("v_tok_s2", [4, 512], F32, 62464)
        self.v_row_s = ar("v_row_s", [4, 256], F32, 64512)
        self.sc_s = ar("sc_s", [4, 512], F32, 65536)
        self.e_s = ar("e_s", [4, 512], F32, 67584)
        self.p_s = ar("p_s", [4, 512], BF16, 69632)
        self.pT_s = ar("pT_s", [128, 16], BF16, 70656)
        self.cl_s = ar("cl_s", [4, 32], F32, 71168)
        self.qs64 = ar("qs64", [64, 16, NSMP], BF16, 71680)
        self.psm = [PBuf(nc, f"psm{i}", i) for i in range(4)]
        self.psa = [PBuf(nc, f"psa{i}", 4 + i) for i in range(2)]
        self.pst = PBuf(nc, "pst", 6, BF16)
        self.pss = PBuf(nc, "pss", 7)
        self.rr = {"main": 0, "aux": 0}

    def ps(self, pool):
        if pool == "main":
            p = self.psm[self.rr["main"] % 4]
            self.rr["main"] += 1
        else:
            p = self.psa[self.rr["aux"] % 2]
            self.rr["aux"] += 1
        return p

    def ev(self):
        self.evi += 1
        return "act" if self.evi % 2 else "dve"

    def mm(self, out, lhsT, rhs, start, stop, inc=None):
        self.S.op("pe", lambda e, o=out.ap, l=lhsT.ap, r=rhs.ap, s=start, p=stop: e.matmul(o, l, r, start=s, stop=p),
                  reads=[lhsT, rhs], writes=[out], inc=(stop if inc is None else inc))

    def transpose(self, out, in_, ident):
        self.S.op("pe", lambda e, o=out.ap, i=in_.ap, d=ident.ap: e.transpose(o, i, d),
                  reads=[in_, ident], writes=[out])

    def act(self, out, in_, func, bias=None, scale=None, accum=None):
        kw = {}
        reads = [in_]
        writes = [out]
        if bias is not None:
            kw["bias"] = bias.ap if _isacc(bias) else bias
            if _isacc(bias):
                reads.append(bias)
        if scale is not None:
            kw["scale"] = scale.ap if _isacc(scale) else scale
            if _isacc(scale):
                reads.append(scale)
        if accum is not None:
            kw["accum_out"] = accum.ap
            writes.append(accum)
        self.S.op("act", lambda e, o=out.ap, i=in_.ap, f=func, kw=kw: e.activation(o, i, f, **kw), reads, writes)

    def copy(self, eng, out, in_):
        if eng == "act":
            self.act(out, in_, AF.Copy)
        else:
            self.S.op(eng, lambda e, o=out.ap, i=in_.ap: e.tensor_copy(o, i), [in_], [out])

    def tt(self, eng, out, a, b, op):
        self.S.op(eng, lambda e, o=out.ap, x=a.ap, y=b.ap, op=op: e.tensor_tensor(o, x, y, op), [a, b], [out])

    def ts(self, eng, out, a, s1, op0, s2=None, op1=None):
        reads = [a]
        v1 = s1.ap if _isacc(s1) else s1
        if _isacc(s1):
            reads.append(s1)
        v2 = s2.ap if _isacc(s2) else s2
        if _isacc(s2):
            reads.append(s2)
        if op1 is None:
            self.S.op(eng, lambda e, o=out.ap, x=a.ap: e.tensor_scalar(o, x, v1, None, op0), reads, [out])
        else:
            self.S.op(eng, lambda e, o=out.ap, x=a.ap: e.tensor_scalar(o, x, v1, v2, op0, op1), reads, [out])

    def stt(self, eng, out, a, s, b, op0, op1):
        reads = [a, b]
        sv = s.ap if _isacc(s) else s
        if _isacc(s):
            reads.append(s)
        self.S.op(eng, lambda e, o=out.ap, x=a.ap, y=b.ap: e.scalar_tensor_tensor(o, x, sv, y, op0, op1), reads, [out])

    def red(self, out, a, op):
        self.S.op("dve", lambda e, o=out.ap, x=a.ap, op=op: e.tensor_reduce(o, x, AX.X, op), [a], [out])

    def recip(self, out, a):
        self.S.op("dve", lambda e, o=out.ap, x=a.ap: e.reciprocal(o, x), [a], [out])

    def memset(self, eng, out, val):
        self.S.op(eng, lambda e, o=out.ap, v=val: e.memset(o, v), [], [out])

    def dma(self, q, out, in_, reads=(), writes=(), **kw):
        self.S.dma(q, out, in_, reads=reads, writes=writes, **kw)

    def _unit_dmas(self, desc, slot):
        kind = desc[0]
        if kind == "win":
            _, l, c0, ncol = desc
            v = self.d_win[l].rearrange("(kt p) n -> p kt n", p=128)
            return [(slot.t[:, 0:16, 0:ncol], v[:, :, c0:c0 + ncol])]
        if kind == "windup":
            _, l, c0 = desc
            v = self.d_win[l].rearrange("(kt p) n -> p kt n", p=128)
            src = v[:, :, c0:c0 + 128].rearrange("p kt (a b) -> p kt a b", a=2)
            dv = slot.t[:, 0:16, 0:256].rearrange("p kt (a d b) -> p kt a d b", a=2, d=2)
            return [(dv[:, :, a, r, :], src[:, :, a, :]) for a in range(2) for r in range(2)]
        if kind == "wbr":
            _, l, n, c0, ncol = desc
            v = self.d_wbr[l, n].rearrange("(kt p) n -> p kt n", p=128)
            return [(slot.t[:, 0:8, 0:ncol], v[:, :, c0:c0 + ncol])]
        if kind == "wout":
            _, l, c0, ncol = desc
            v = self.d_wout[l].rearrange("(kt p) n -> p kt n", p=128)
            return [(slot.t[:, 0:16, 0:ncol], v[:, :, c0:c0 + ncol])]
        if kind == "wup":
            _, l, c0, ncol = desc
            v = self.d_wup[l].rearrange("(kt p) n -> p kt n", p=128)
            return [(slot.t[:, 0:16, 0:ncol], v[:, :, c0:c0 + ncol])]
        if kind == "wdn":
            _, l, k0, c0, ncol = desc
            v = self.d_wdn[l].rearrange("(kt p) n -> p kt n", p=128)
            return [(slot.t[:, 0:16, 0:ncol], v[:, k0:k0 + 16, c0:c0 + ncol])]
        raise ValueError(kind)

    def _issue(self, i):
        desc = self.wseq[i]
        slot = self.wslot[i % 3]
        for dst, src in self._unit_dmas(desc, slot):
            self.dma("pool", dst, src, writes=[slot[:, :, :]])

    def wget(self, desc):
        i = self.widx
        self.widx += 1
        if self.wseq is None:
            self.wrec.append(desc)
            slot = self.wslot[i % 3]
            for dst, src in self._unit_dmas(desc, slot):
                self.dma("pool", dst, src, writes=[slot[:, :, :]])
            return slot
        assert self.wseq[i] == desc, (i, self.wseq[i], desc)
        while self.wissued < min(len(self.wseq), i + 3):
            self._issue(self.wissued)
            self.wissued += 1
        return self.wslot[i % 3]

    def proj(self, mk, c0, c1, src, KT, N=TT, evF=None, evT=None, srcs=None, evFs=None, evTs=None, ntb=4):
        for u0 in range(c0, c1, 256):
            ncol = min(256, c1 - u0)
            slot = self.wget(mk(u0, ncol))
            if evF is not None:
                for m0 in range(0, ncol, 128):
                    mc = min(128, ncol - m0)
                    p = self.ps("main")
                    for kt in range(KT):
                        self.mm(p[0:mc, 0:N], slot[:, kt, m0:m0 + mc], src[:, kt, 0:N], kt == 0, kt == KT - 1)
                    evF(p, u0 + m0, mc)
                    if srcs is not None and evFs is not None:
                        q = self.pss
                        for kt in range(KT):
                            self.mm(q[0:mc, 0:NSMP], slot[:, kt, m0:m0 + mc], srcs[:, kt, 0:NSMP], kt == 0, kt == KT - 1)
                        evFs(q, u0 + m0, mc)
            if evT is not None:
                for tb in range(ntb):
                    p = self.ps("main")
                    for kt in range(KT):
                        self.mm(p[:, 0:ncol], src[:, kt, tb * 128:(tb + 1) * 128], slot[:, kt, 0:ncol], kt == 0, kt == KT - 1)
                    evT(p, tb, u0, ncol)
            if srcs is not None and evTs is not None:
                q = self.pss
                for kt in range(KT):
                    self.mm(q[0:NSMP, 0:ncol], srcs[:, kt, 0:NSMP], slot[:, kt, 0:ncol], kt == 0, kt == KT - 1)
                evTs(q, u0, ncol)

    def load_consts(self):
        self.dma("sp", self.cst.t[:, :], self.d_cst[:, :], writes=[self.cst[:, :]])
        self.copy("act", self.identb[:, :], self.cst[:, K_ID:K_ID + 128])
        self.memset("dve", self.cols[:, 0:1], EPS)
        self.ones = self.cst.acc(self.cst.t[:, K_ONES:K_ONES + 128], (slice(None), slice(K_ONES, K_ONES + 128)))

    def cs(self, c0, n, p=128):
        return self.cst[0:p, c0:c0 + n]

    def load_prm(self, l, gi):
        b = self.prm2[gi % 2]
        self.dma("sp", b.t[:, :], self.d_prm[l], writes=[b[:, :]])
        self.dma("sp", self.wlr.t[:, :], self.d_wlr[l], writes=[self.wlr[:, :]])
        self.prm = b

    def norm(self, x, nkt, N, gbase, out, inv_d):
        p = self.ps("aux")
        for kt in range(nkt):
            sq = self.sqt[kt % 2]
            self.act(sq[:, 0:N], x[:, kt, 0:N], AF.Square)
            self.mm(p[:, 0:N], self.ones, sq[:, 0:N], kt == 0, kt == nkt - 1)
        self.act(self.rs[:, 0:N], p[:, 0:N], AF.Sqrt, bias=self.cols[:, 0:1], scale=inv_d)
        self.recip(self.rs[:, 0:N], self.rs[:, 0:N])
        for kt in range(nkt):
            if callable(out):
                out(kt)
            else:
                self.stt("dve", out[:, kt, 0:N], x[:, kt, 0:N], self.prm[:, gbase + kt:gbase + kt + 1],
                         self.rs[:, 0:N], ALU.mult, ALU.mult)

    def gla(self, t, l, ws):
        prm = self.prm
        mk = lambda c, n: ("win", l, c, n)
        S_ = self.Sst[l]
        gs = self.gsm
        GLR_S, Q_S, K_S, A_S, OT_S, NB = 0, 8, 24, 40, 56, 88
        def ev_glr(p, c, mc):
            self.copy("act", self.glr[0:16, 0:TT], p[0:16, 0:TT])
        def ev_glr_s(q, c, mc):
            self.copy("act", gs[0:16, GLR_S:GLR_S + NSMP], q[0:16, 0:NSMP])
        self.proj(mk, C_GLR, C_GLR + 16, self.hT, 16, evF=ev_glr, srcs=self.hTs if ws else None, evFs=ev_glr_s)
        for tb in range(4):
            p = self.ps("aux")
            self.mm(p[:, 0:512], self.glr[0:16, tb * 128:(tb + 1) * 128], self.wlr[0:16, 0:512], True, True)
            self.tt("dve", self.sp_tok[:, tb, :], p[:, 0:512], prm[:, P_BLRBC:P_BLRBC + 512], ALU.add)
            self.act(self.sp_tok[:, tb, :], self.sp_tok[:, tb, :], AF.Exp, scale=-1.0)
            self.act(self.sp_tok[:, tb, :], self.sp_tok[:, tb, :], AF.Ln, bias=1.0)
        for tb in range(4):
            p = self.ps("aux")
            self.mm(p[:, 0:512], self.cs(K_GT, 128), self.sp_tok[:, tb, :], True, True)
            self.act(self.ekd[:, tb, :], p[:, 0:512], AF.Exp)
            for h in range(4):
                p = self.ps("aux")
                self.mm(p[:, 0:128], self.sp_tok[:, tb, h * 128:(h + 1) * 128], self.cs(K_TRI, 128), True, True)
                self.act(self.expb[:, h, tb * 128:(tb + 1) * 128], p[:, 0:128], AF.Exp)
                self.act(self.expnb[:, h, tb * 128:(tb + 1) * 128], p[:, 0:128], AF.Exp, scale=-1.0)
        for h in range(4):
            src = self.expb.acc(self.expb.t[:, h, 63:TT:64], (slice(None), h))
            self.copy("dve", self.gcol[:, h, 0:8], src)
        def ev_kF(p, c, mc):
            h = (c - C_K) // 128
            self.tt("dve", self.kT[:, h, :], p[:, 0:TT], self.expnb[:, h, :], ALU.mult)
        def ev_kT(p, tb, u0, ncol):
            cc = u0 - C_K
            self.tt("dve", self.kd[:, tb, cc:cc + ncol], p[:, 0:ncol], self.ekd[:, tb, cc:cc + ncol], ALU.mult)
        def ev_kF_s(q, c, mc):
            h = (c - C_K) // 128
            self.copy("act", gs[:, K_S + 4 * h:K_S + 4 * h + NSMP], q[:, 0:NSMP])
        self.proj(mk, C_K, C_K + 512, self.hT, 16, evF=ev_kF, evT=ev_kT, srcs=self.hTs if ws else None, evFs=ev_kF_s)
        def ev_q(p, c, mc):
            h = (c - C_Q) // 128
            self.stt("dve", self.qT[:, h, :], p[:, 0:TT], 128.0 ** -0.5, self.expb[:, h, :], ALU.mult, ALU.mult)
        def ev_q_s(q, c, mc):
            h = (c - C_Q) // 128
            self.act(gs[:, Q_S + 4 * h:Q_S + 4 * h + NSMP], q[:, 0:NSMP], AF.Copy, scale=128.0 ** -0.5)
        self.proj(mk, C_Q, C_Q + 512, self.hT, 16, evF=ev_q, srcs=self.hTs if ws else None, evFs=ev_q_s)
        def ev_vT(p, tb, u0, ncol):
            cc = u0 - C_V
            self.copy(self.ev(), self.v_tok[:, tb, cc:cc + ncol], p[:, 0:ncol])
        def ev_vT_s(q, u0, ncol):
            cc = u0 - C_V
            self.copy("act", self.v_tok_s[0:NSMP, cc:cc + ncol], q[0:NSMP, 0:ncol])
        self.proj(mk, C_V, C_V + 1024, self.hT, 16, evT=ev_vT, srcs=self.hTs if ws else None, evTs=ev_vT_s)
        def ev_gr(p, c, mc):
            m = (c - C_GR) // 128
            self.act(self.gr_s[:, m, :], p[:, 0:TT], AF.Silu)
        def ev_gr_s(q, c, mc):
            m = (c - C_GR) // 128
            self.act(self.grss[:, m, 0:NSMP], q[:, 0:NSMP], AF.Silu)
        self.proj(mk, C_GR, C_GR + 1024, self.hT, 16, evF=ev_gr, srcs=self.hTs if ws else None, evFs=ev_gr_s)
        if t == 0:
            self.memset("dve", S_[:, :, :], 0.0)
        self.copy("act", self.Sbf[:, :, :], S_[:, :, :])
        po = [self.psm[0], self.psm[1]]
        for tb in range(4):
            for h in range(4):
                pA = self.ps("aux")
                blk = slice(tb * 128, (tb + 1) * 128)
                self.mm(pA[:, 0:128], self.kT[:, h, blk], self.qT[:, h, blk], True, True)
                self.tt("dve", self.ATm[h][:, :], pA[:, 0:128], self.cs(K_BD, 128), ALU.mult)
            for ch in range(2):
                rsl = slice(ch * 64, ch * 64 + 64)
                tok = slice(tb * 128 + ch * 64, tb * 128 + ch * 64 + 64)
                ci = tb * 2 + ch
                for h in range(4):
                    pb = po[h // 2]
                    for half in range(2):
                        o0 = (h % 2) * 256 + half * 128 + ch * 64
                        self.mm(pb[:, o0:o0 + 64], self.v_tok[rsl, tb, h * 256 + half * 128:h * 256 + half * 128 + 128],
                                self.ATm[h][rsl, ch * 64:ch * 64 + 64], True, False)
                        self.mm(pb[:, o0:o0 + 64], self.Sbf[:, h, half * 128:(half + 1) * 128],
                                self.qT[:, h, tok], False, True)
                    pU = self.psm[2 + (h % 2)]
                    self.mm(pU[:, 0:256], self.kd[rsl, tb, h * 128:(h + 1) * 128],
                            self.v_tok[rsl, tb, h * 256:(h + 1) * 256], True, True)
                    self.stt("dve", S_[:, h, :], S_[:, h, :], self.gcol[:, h, ci:ci + 1], pU[:, 0:256], ALU.mult, ALU.add)
                    self.copy("act", self.Sbf[:, h, :], S_[:, h, :])
            for h in range(4):
                pb = po[h // 2]
                for half in range(2):
                    o0 = (h % 2) * 256 + half * 128
                    self.copy(self.ev(), self.oT[:, h * 2 + half, tb * 128:(tb + 1) * 128], pb[:, o0:o0 + 128])
        if t == self.ntile - 1:
            self.dma("sp", self.o_sgp[l].rearrange("h k v -> k h v"), S_.t[:, :, :], reads=[S_[:, :, :]])
        self.gla_norm(self.oT, TT, self.gr_s, self.out_a)
        if ws:
            self.ts("dve", gs[:, NB:NB + 4], prm[:, P_BLR:P_BLR + 4], -1.0, ALU.mult)
            for h in range(4):
                q = self.pss
                self.mm(q[:, 0:NSMP], self.wlr[0:16, h * 128:(h + 1) * 128], gs[0:16, GLR_S:GLR_S + NSMP], True, True)
                a_h = gs[:, A_S + 4 * h:A_S + 4 * h + NSMP]
                self.act(a_h, q[:, 0:NSMP], AF.Exp, bias=gs[:, NB + h:NB + h + 1], scale=-1.0)
                self.act(a_h, a_h, AF.Ln, bias=1.0)
                self.act(a_h, a_h, AF.Exp, scale=-1.0 / 16.0)
            n = 0
            for b in range(NSMP):
                for h in range(4):
                    st = self.stg[n % 2]
                    n += 1
                    self.dma("sp", st.t[:, :], self.d_sg[l, b, h], writes=[st[:, :]])
                    q = self.ps("aux")
                    self.mm(q[:, 0:256], self.cst[0:NSMP, K_SEL + b * 128:K_SEL + (b + 1) * 128],
                            self.v_tok_s[0:NSMP, h * 256:(h + 1) * 256], True, True)
                    self.ts("dve", st[:, :], st[:, :], gs[:, A_S + 4 * h + b:A_S + 4 * h + b + 1], ALU.mult)
                    self.stt("dve", st[:, :], q[:, 0:256], gs[:, K_S + 4 * h + b:K_S + 4 * h + b + 1], st[:, :],
                             ALU.mult, ALU.add)
                    self.dma("sp", self.o_sgs[l, b, h], st.t[:, :], reads=[st[:, :]])
                    q2 = self.pss
                    for half in range(2):
                        self.mm(q2[:, half:half + 1], st[:, half * 128:(half + 1) * 128],
                                gs[:, Q_S + 4 * h + b:Q_S + 4 * h + b + 1], True, True)
                    oslot = gs.acc(gs.t[:, OT_S + (h * 2) * 4 + b:OT_S + (h * 2 + 2) * 4 + b:4],
                                   (slice(None), slice(OT_S, OT_S + 32)))
                    self.copy("dve", oslot, q2[:, 0:2])
            oTs = _View3(gs, OT_S, 8, NSMP)
            self.gla_norm(oTs, NSMP, self.grss, self.oas)

    def gla_norm(self, oT, N, gr, out):
        prm = self.prm
        for h in range(4):
            p = self.ps("aux")
            for half in range(2):
                sq = self.sqt[half]
                self.act(sq[:, 0:N], oT[:, h * 2 + half, 0:N], AF.Square)
                self.mm(p[:, 0:N], self.ones, sq[:, 0:N], half == 0, half == 1)
            self.act(self.rs[:, 0:N], p[:, 0:N], AF.Sqrt, bias=self.cols[:, 0:1], scale=1.0 / 256.0)
            self.recip(self.rs[:, 0:N], self.rs[:, 0:N])
            for half in range(2):
                m = h * 2 + half
                self.stt("dve", self.tmpf[:, 0:N], oT[:, m, 0:N], prm[:, P_GN + half:P_GN + half + 1],
                         self.rs[:, 0:N], ALU.mult, ALU.mult)
                self.tt("dve", out[:, m, 0:N], self.tmpf[:, 0:N], gr[:, m, 0:N], ALU.mult)

    def sample_window_loads(self, l):
        lst = [lambda: self.dma("pool", self.kwinT.t[:, :, :, 0:127], self.d_ckT[l][:, :, :, 1:128],
                                writes=[self.kwinT[:, :, :, :]])]
        vsrc = self.d_cv[l][:, 1:128, :].rearrange("b j (k d) -> j b k d", k=4)
        vdst = self.vwin.t[0:127, :, :].rearrange("j b (k r d) -> j b k r d", k=4, r=2)
        for b in range(NSMP):
            for r in range(2):
                lst.append(lambda b=b, r=r: self.dma("pool", vdst[:, b, :, r, :], vsrc[:, b, :, :],
                                                     writes=[self.vwin[:, b, :]]))
        return lst

    def conv(self, t, l, ws):
        prm = self.prm
        mk = lambda c, n: ("win", l, c, n)
        up = self.uprev[l]
        cm = self.csm
        CC_S, CH_S, PRV, USM, T_S = 0, 32, 64, 128, 192
        if t == 0:
            self.memset("dve", up[:, :, :], 0.0)
        if ws:
            self.dma("sp", cm.t[:, PRV:PRV + 64], self.d_sc[l].rearrange("p k b j -> p (k b j)"),
                     writes=[cm[:, PRV:PRV + 64]])
        pend = self.sample_window_loads(l) if ws else []
        for pr_ in range(4):
            for _ in range(3):
                if pend:
                    pend.pop(0)()
            def ev_cc(p, c, mc):
                m = (c - C_CC) // 128
                self.copy("act", self.ccs[:, m % 2, :], p[:, 0:TT])
            def ev_cc_s(q, c, mc):
                m = (c - C_CC) // 128
                self.copy("act", cm[:, CC_S + 4 * m:CC_S + 4 * m + NSMP], q[:, 0:NSMP])
            self.proj(mk, C_CC + pr_ * 256, C_CC + pr_ * 256 + 256, self.hT, 16, evF=ev_cc,
                      srcs=self.hTs if ws else None, evFs=ev_cc_s)
            def ev_ch(p, c, mc):
                m = (c - C_CH) // 128
                self.copy("act", self.ubuf[:, m % 2, 0:2], up[:, m, 0:2])
                self.tt("dve", self.ubuf[:, m % 2, 2:TT + 2], p[:, 0:TT], self.ccs[:, m % 2, :], ALU.mult)
                self.copy("act", up[:, m, 0:2], self.ubuf[:, m % 2, TT:TT + 2])
            def ev_ch_s(q, c, mc):
                m = (c - C_CH) // 128
                self.tt("dve", cm[:, USM + 4 * m:USM + 4 * m + NSMP], q[:, 0:NSMP],
                        cm[:, CC_S + 4 * m:CC_S + 4 * m + NSMP], ALU.mult)
            self.proj(mk, C_CH + pr_ * 256, C_CH + pr_ * 256 + 256, self.hT, 16, evF=ev_ch,
                      srcs=self.hTs if ws else None, evFs=ev_ch_s)
            def ev_cb(p, c, mc):
                m = (c - C_CB) // 128
                u = self.ubuf
                w = lambda j: prm[:, P_CW + j * 8 + m:P_CW + j * 8 + m + 1]
                c0, c1 = self.ct
                self.ts("dve", c0[:, :], u[:, m % 2, 2:TT + 2], w(2), ALU.mult)
                self.stt("dve", c1[:, :], u[:, m % 2, 1:TT + 1], w(1), c0[:, :], ALU.mult, ALU.add)
                self.stt("dve", c0[:, :], u[:, m % 2, 0:TT], w(0), c1[:, :], ALU.mult, ALU.add)
                self.tt("dve", self.out_b[:, m, :], p[:, 0:TT], c0[:, :], ALU.mult)
            def ev_cb_s(q, c, mc):
                m = (c - C_CB) // 128
                w = lambda j: prm[:, P_CW + j * 8 + m:P_CW + j * 8 + m + 1]
                pv = lambda j: cm.acc(cm.t[:, PRV + m * 8 + j:PRV + m * 8 + 8 + j:2],
                                      (slice(None), slice(PRV + m * 8, PRV + m * 8 + 8)))
                us = cm[:, USM + 4 * m:USM + 4 * m + NSMP]
                ta = cm[:, T_S:T_S + NSMP]
                tb_ = cm[:, T_S + 4:T_S + 4 + NSMP]
                self.ts("dve", ta, us, w(2), ALU.mult)
                self.stt("dve", tb_, pv(1), w(1), ta, ALU.mult, ALU.add)
                self.stt("dve", ta, pv(0), w(0), tb_, ALU.mult, ALU.add)
                self.tt("dve", self.obs[:, m, 0:NSMP], q[:, 0:NSMP], ta, ALU.mult)
                o0 = cm.acc(cm.t[:, 256 + m * 8:256 + m * 8 + 8:2], (slice(None), slice(256 + m * 8, 256 + m * 8 + 8)))
                o1 = cm.acc(cm.t[:, 256 + m * 8 + 1:256 + m * 8 + 9:2], (slice(None), slice(256 + m * 8, 256 + m * 8 + 8)))
                self.copy("dve", o0, pv(1))
                self.copy("dve", o1, us)
            self.proj(mk, C_CB + pr_ * 256, C_CB + pr_ * 256 + 256, self.hT, 16, evF=ev_cb,
                      srcs=self.hTs if ws else None, evFs=ev_cb_s)
        if t == self.ntile - 1:
            self.dma("sp", self.o_scp[l], up.t[:, :, :], reads=[up[:, :, :]])
        if ws:
            self.dma("sp", self.o_scs[l].rearrange("p k b j -> p (k b j)"), cm.t[:, 256:320], reads=[cm[:, 256:320]])

    def swa(self, t, l, ws, bg=None):
        prm = self.prm
        last = (t == self.ntile - 1)
        kp, vp = self.kprev[l], self.vprev[l]
        if t == 0:
            self.memset("dve", kp[:, :, :], 0.0)
            self.memset("dve", vp[:, :], 0.0)
        ws0 = ws
        ws = ws0 and DBG_SWS >= 2
        if ws0 and DBG_SWS >= 1:
            self.dma("sp", self.o_cks[l][:, 0:127, :], self.d_ck[l][:, 1:128, :])
            self.dma("sp", self.o_cvs[l][:, 0:127, :], self.d_cv[l][:, 1:128, :])
        def ev_sq(p, c, mc):
            m = (c - C_SQ) // 128
            self.act(self.qsT[:, m, :], p[:, 0:TT], AF.Copy, scale=0.125)
        for u0 in range(C_SQ, C_SQ + 1024, 256):
            slot = self.wget(("win", l, u0, 256))
            for m0 in range(0, 256, 128):
                p = self.ps("main")
                for kt in range(16):
                    self.mm(p[:, 0:TT], slot[:, kt, m0:m0 + 128], self.hT[:, kt, :], kt == 0, kt == 15)
                ev_sq(p, u0 + m0, 128)
            if ws and (DBG_M & 1):
                for hh in range(4):
                    hq = (u0 - C_SQ) // 64 + hh
                    q = self.pss
                    for kt in range(16):
                        self.mm(q[0:64, 0:NSMP], slot[:, kt, hh * 64:(hh + 1) * 64], self.hTs[:, kt, 0:NSMP], kt == 0, kt == 15)
                    self.act(self.qs64[0:64, hq, 0:NSMP], q[0:64, 0:NSMP], AF.Copy, scale=0.125)
        self.copy("act", self.kdupT[:, :, 0:128], kp[:, :, :])
        for pair in range(2):
            slot = self.wget(("windup", l, C_SK + pair * 128))
            for kvl in range(2):
                kvh = pair * 2 + kvl
                p = self.ps("main")
                for kt in range(16):
                    self.mm(p[:, 0:TT], slot[:, kt, kvl * 128:(kvl + 1) * 128], self.hT[:, kt, :], kt == 0, kt == 15)
                self.copy(self.ev(), self.kdupT[:, kvh, 128:640], p[:, 0:TT])
                if ws and (DBG_M & 2):
                    q = self.pss
                    for kt in range(16):
                        self.mm(q[0:64, 0:NSMP], slot[:, kt, kvl * 128:kvl * 128 + 64], self.hTs[:, kt, 0:NSMP], kt == 0, kt == 15)
                    dst = self.kwinT.acc(self.kwinT.t[0:64, :, kvh, 127], (slice(None),))
                    self.copy("act", dst, q[0:64, 0:NSMP])
            if last:
                p = self.ps("main")
                for kt in range(16):
                    self.mm(p[:, 0:256], self.hT[:, kt, 384:512], slot[:, kt, 0:256], kt == 0, kt == 15)
                src = p.acc(p.t[:, 0:256].rearrange("p (a r d) -> p a r d", a=2, r=2)[:, :, 0, :], 0, 256)
                dst = self.kout.acc(self.kout.t[:, pair * 128:(pair + 1) * 128].rearrange("p (a d) -> p a d", a=2),
                                    (slice(None), slice(pair * 128, pair * 128 + 128)))
                self.copy("dve", dst, src)
            if ws and (DBG_M & 4):
                q = self.pss
                for kt in range(16):
                    self.mm(q[0:NSMP, 0:256], self.hTs[:, kt, 0:NSMP], slot[:, kt, 0:256], kt == 0, kt == 15)
                src = q.acc(q.t[0:NSMP, 0:256].rearrange("p (a r d) -> p a r d", a=2, r=2)[:, :, 0, :], 0, 256)
                dst = self.k_tok_s.acc(self.k_tok_s.t[0:NSMP, pair * 128:(pair + 1) * 128].rearrange("p (a d) -> p a d", a=2),
                                       (slice(None), slice(pair * 128, pair * 128 + 128)))
                self.copy("dve", dst, src)
        if last:
            self.dma("sp", self.o_ckp[l], self.kout.t[:, :], reads=[self.kout[:, :]])
        if ws and (DBG_M & 4):
            self.dma("sp", self.o_cks[l][:, 127, :], self.k_tok_s.t[0:NSMP, :], reads=[self.k_tok_s[:, :]])
        self.copy("act", self.vdup[:, 0, :], vp[:, :])
        for pair in range(2):
            slot = self.wget(("windup", l, C_SV + pair * 128))
            for tb in range(4):
                p = self.ps("main")
                for kt in range(16):
                    self.mm(p[:, 0:256], self.hT[:, kt, tb * 128:(tb + 1) * 128], slot[:, kt, 0:256], kt == 0, kt == 15)
                self.copy(self.ev(), self.vdup[:, tb + 1, pair * 256:(pair + 1) * 256], p[:, 0:256])
                if last and tb == 3:
                    src = p.acc(p.t[:, 0:256].rearrange("p (a r d) -> p a r d", a=2, r=2)[:, :, 0, :], 0, 256)
                    dst = self.vout.acc(self.vout.t[:, pair * 128:(pair + 1) * 128].rearrange("p (a d) -> p a d", a=2),
                                        (slice(None), slice(pair * 128, pair * 128 + 128)))
                    self.copy("dve", dst, src)
            if ws and (DBG_M & 8):
                q = self.pss
                for kt in range(16):
                    self.mm(q[0:NSMP, 0:256], self.hTs[:, kt, 0:NSMP], slot[:, kt, 0:256], kt == 0, kt == 15)
                src = q.acc(q.t[0:NSMP, 0:256].rearrange("p (a r d) -> p a r d", a=2, r=2)[:, :, 0, :], 0, 256)
                dst = self.v_row_s.acc(self.v_row_s.t[0:NSMP, pair * 128:(pair + 1) * 128].rearrange("p (a d) -> p a d", a=2),
                                       (slice(None), slice(pair * 128, pair * 128 + 128)))
                self.copy("dve", dst, src)
        if last:
            self.dma("sp", self.o_cvp[l], self.vout.t[:, :], reads=[self.vout[:, :]])
        if ws and (DBG_M & 8):
            self.dma("sp", self.o_cvs[l][:, 127, :], self.v_row_s.t[0:NSMP, :], reads=[self.v_row_s[:, :]])
        if ws and (DBG_M & 16):
            for b in range(NSMP):
                dstv = self.vwin.t[127:128, b, :].rearrange("p (k r d) -> p k r d", k=4, r=2)
                srcv = self.v_row_s.t[b:b + 1, :].rearrange("p (k d) -> p k d", k=4)
                for r in range(2):
                    self.dma("pool", dstv[:, :, r, :], srcv, reads=[self.v_row_s[:, :]], writes=[self.vwin[:, b, :]])
        items = [(tb, hq) for tb in range(4) for hq in range(16)]
        NI = len(items)

        def geo(i):
            tb, hq = items[i]
            kvh, m, half = hq // 4, hq // 2, hq % 2
            return tb, hq, kvh, m, slice(half * 64, half * 64 + 64), slice(tb * 128, (tb + 1) * 128)

        def st1(i):
            tb, hq, kvh, m, pr, blk = geo(i)
            p = self.psm[i % 2]
            self.mm(p[:, 0:256], self.qsT[pr, m, blk], self.kdupT[pr, kvh, tb * 128:tb * 128 + 256], True, True)

        def st2a(i):
            tb, hq, kvh, m, pr, blk = geo(i)
            first = (t == 0 and tb == 0)
            Dp = self.cs(K_DPF if first else K_DP, 256)
            p = self.psm[i % 2]
            sb, pex, sc = self.sb[i % 2], self.pex[i % 2], self.sc8[i % 4]
            sink = prm[:, P_SINK + hq:P_SINK + hq + 1]
            self.stt("dve", sb[:, :], Dp, -SLOPES[hq], p[:, 0:256], ALU.mult, ALU.add)
            self.red(sc[:, 0:1], sb[:, :], ALU.max)
            self.tt("dve", sc[:, 1:2], sc[:, 0:1], sink, ALU.max)
            self.ts("dve", sc[:, 2:3], sc[:, 1:2], -1.0, ALU.mult)
            self.act(pex[:, :], sb[:, :], AF.Exp, bias=sc[:, 2:3], accum=sc[:, 3:4])
            self.act(sc[:, 4:5], sink, AF.Exp, bias=sc[:, 2:3])

        def st2b(i):
            pex, pn, sc = self.pex[i % 2], self.pn[i % 4], self.sc8[i % 4]
            self.tt("dve", sc[:, 5:6], sc[:, 3:4], sc[:, 4:5], ALU.add)
            self.recip(sc[:, 6:7], sc[:, 5:6])
            self.act(pn[:, :], pex[:, :], AF.Copy, scale=sc[:, 6:7])

        def st3(i):
            pn, pTs = self.pn[i % 4], self.pTsb[i % 4]
            for jb in range(2):
                self.transpose(self.pst[:, jb * 128:(jb + 1) * 128], pn[:, jb * 128:(jb + 1) * 128], self.identb[:, :])
            self.copy("act", pTs[:, :], self.pst[:, 0:256])

        def st4(i):
            tb, hq, kvh, m, pr, blk = geo(i)
            pTs = self.pTsb[i % 4]
            p2 = self.psa[i % 2]
            for jb in range(2):
                self.mm(p2[:, 0:128], self.vdup[:, tb + jb, kvh * 128:(kvh + 1) * 128], pTs[:, jb * 128:(jb + 1) * 128],
                        jb == 0, jb == 1)
            self.copy("act" if i % 2 else "dve", self.out_c[pr, m, blk], p2[pr, 0:128])

        for s_ in range(NI + 4):
            if s_ < NI:
                st1(s_)
            if 0 <= s_ - 1 < NI:
                st2a(s_ - 1)
            if 0 <= s_ - 2 < NI:
                st2b(s_ - 2)
            if 0 <= s_ - 3 < NI:
                st3(s_ - 3)
            if 0 <= s_ - 4 < NI:
                st4(s_ - 4)
            if bg is not None:
                next(bg, None)
        self.copy("act", kp[:, :, :], self.kdupT[:, :, 512:640])
        self.copy("act", vp[:, :], self.vdup[:, 4, :])
        if ws0 and DBG_SWS >= 3:
            cl = self.cl_s
            for b in range(NSMP):
                q = self.pss
                for kvh in range(4):
                    lhs = self.qs64.acc(self.qs64.t[0:64, kvh * 4:(kvh + 1) * 4, b], (slice(None),))
                    self.mm(q[0:4, kvh * 128:(kvh + 1) * 128], lhs, self.kwinT[0:64, b, kvh, :], True, True)
                self.tt("dve", self.sc_s[0:4, :], q[0:4, 0:512], self.cst[0:4, K_BS:K_BS + 512], ALU.add)
                v3 = self.sc_s.acc(self.sc_s.t[0:4, :].rearrange("p (k j) -> p k j", k=4), (slice(None),))
                self.red(cl[0:4, 0:4], v3, ALU.max)
                sk = prm[0:4, P_SINKS:P_SINKS + 4]
                self.tt("dve", cl[0:4, 4:8], cl[0:4, 0:4], sk, ALU.max)
                self.ts("dve", cl[0:4, 8:12], cl[0:4, 4:8], -1.0, ALU.mult)
                for kvh in range(4):
                    self.act(self.e_s[0:4, kvh * 128:(kvh + 1) * 128], self.sc_s[0:4, kvh * 128:(kvh + 1) * 128], AF.Exp,
                             bias=cl[0:4, 8 + kvh:9 + kvh], accum=cl[0:4, 12 + kvh:13 + kvh])
                self.tt("dve", cl[0:4, 16:20], sk, cl[0:4, 8:12], ALU.add)
                self.act(cl[0:4, 16:20], cl[0:4, 16:20], AF.Exp)
                self.tt("dve", cl[0:4, 20:24], cl[0:4, 12:16], cl[0:4, 16:20], ALU.add)
                self.recip(cl[0:4, 24:28], cl[0:4, 20:24])
                for kvh in range(4):
                    self.ts("dve", self.p_s[0:4, kvh * 128:(kvh + 1) * 128], self.e_s[0:4, kvh * 128:(kvh + 1) * 128],
                            cl[0:4, 24 + kvh:25 + kvh], ALU.mult)
                for kvh in range(4):
                    self.transpose(self.pst[:, kvh * 4:kvh * 4 + 4], self.p_s[0:4, kvh * 128:(kvh + 1) * 128],
                                   self.identb[0:4, 0:4])
                self.copy("act", self.pT_s[:, 0:16], self.pst[:, 0:16])
                for kvh in range(4):
                    q2 = self.ps("aux")
                    self.mm(q2[:, 0:4], self.vwin[:, b, kvh * 128:(kvh + 1) * 128], self.pT_s[:, kvh * 4:kvh * 4 + 4], True, True)
                    for g in range(4):
                        pr = slice((g % 2) * 64, (g % 2) * 64 + 64)
                        self.copy("dve", self.ocs[pr, kvh * 2 + g // 2, b:b + 1], q2[pr, g:g + 1])

    def merge_gen(self, t, l, ws, nlist):
        outs = [self.out_a, self.out_b, self.out_c]
        outs_s = [self.oas, self.obs, self.ocs]
        sm = self.smisc
        sigb, accb, tmb = (self.sig, self.accm, self.tmul) if ws else (self.sigB, self.accmB, self.tmulB)
        for mp in range(8):
            for n in nlist:
                gslot = self.wget(("win", l, C_G + n * 2048 + mp * 256, 256))
                for m2 in range(2):
                    p = self.psm[2 + self.mcnt % 2]
                    self.mcnt += 1
                    for kt in range(16):
                        self.mm(p[:, 0:TT], gslot[:, kt, m2 * 128:(m2 + 1) * 128], self.hT[:, kt, :], kt == 0, kt == 15)
                    yield
                    self.act(sigb[m2][:, :], p[:, 0:TT], AF.Sigmoid)
                    if ws:
                        q = self.pss
                        for kt in range(16):
                            self.mm(q[:, 0:NSMP], gslot[:, kt, m2 * 128:(m2 + 1) * 128], self.hTs[:, kt, 0:NSMP], kt == 0, kt == 15)
                        self.act(sm[:, m2 * 4:m2 * 4 + NSMP], q[:, 0:NSMP], AF.Sigmoid)
                bslot = self.wget(("wbr", l, n, mp * 256, 256))
                for m2 in range(2):
                    m = mp * 2 + m2
                    p = self.psm[2 + self.mcnt % 2]
                    self.mcnt += 1
                    for kt in range(8):
                        self.mm(p[:, 0:TT], bslot[:, kt, m2 * 128:(m2 + 1) * 128], outs[n][:, kt, :], kt == 0, kt == 7)
                    yield
                    ac = accb[m2]
                    if n == 0:
                        self.tt("dve", ac[:, :], sigb[m2][:, :], p[:, 0:TT], ALU.mult)
                    elif n == 1:
                        self.tt("dve", tmb[m2][:, :], sigb[m2][:, :], p[:, 0:TT], ALU.mult)
                        self.tt("dve", self.ypre[:, m, :], ac[:, :], tmb[m2][:, :], ALU.add)
                    else:
                        self.tt("dve", tmb[m2][:, :], sigb[m2][:, :], p[:, 0:TT], ALU.mult)
                        self.tt("dve", self.ypre[:, m, :], self.ypre[:, m, :], tmb[m2][:, :], ALU.add)
                    if ws:
                        q = self.pss
                        for kt in range(8):
                            self.mm(q[:, 0:NSMP], bslot[:, kt, m2 * 128:(m2 + 1) * 128], outs_s[n][:, kt, 0:NSMP], kt == 0, kt == 7)
                        acs = sm[:, 16 + m2 * 4:16 + m2 * 4 + NSMP]
                        tms = sm[:, 32 + m2 * 4:32 + m2 * 4 + NSMP]
                        sgs = sm[:, m2 * 4:m2 * 4 + NSMP]
                        if n == 0:
                            self.tt("dve", acs, sgs, q[:, 0:NSMP], ALU.mult)
                        elif n == 1:
                            self.tt("dve", tms, sgs, q[:, 0:NSMP], ALU.mult)
                            self.tt("dve", self.ypres[:, m, 0:NSMP], acs, tms, ALU.add)
                        else:
                            self.tt("dve", tms, sgs, q[:, 0:NSMP], ALU.mult)
                            self.tt("dve", self.ypres[:, m, 0:NSMP], self.ypres[:, m, 0:NSMP], tms, ALU.add)

    def wout(self, t, l, ws):
        def ev(p, c, mc):
            m = c // 128
            self.tt("dve", self.xT[:, m, :], self.xT[:, m, :], p[:, 0:TT], ALU.add)
        def ev_s(q, c, mc):
            m = c // 128
            self.tt("dve", self.xs[:, m, 0:NSMP], self.xs[:, m, 0:NSMP], q[:, 0:NSMP], ALU.add)
        self.proj(lambda c, n: ("wout", l, c, n), 0, D, self.ypre, 16, evF=ev,
                  srcs=self.ypres if ws else None, evFs=ev_s)

    def mlp(self, t, l, ws):
        sm = self.smisc
        def ev(p, c, mc):
            m = c // 128
            r = self.relu[m % 2]
            self.act(r[:, :], p[:, 0:TT], AF.Relu)
            self.tt("dve", self.hid[:, m, :], r[:, :], r[:, :], ALU.mult)
        def ev_s(q, c, mc):
            m = c // 128
            r = sm[:, 48:48 + NSMP]
            self.act(r, q[:, 0:NSMP], AF.Relu)
            self.tt("dve", self.hids[:, m, 0:NSMP], r, r, ALU.mult)
        self.proj(lambda c, n: ("wup", l, c, n), 0, 4 * D, self.hT, 16, evF=ev,
                  srcs=self.hTs if ws else None, evFs=ev_s)
        for u0 in range(0, D, 256):
            ps2 = [self.ps("main"), self.ps("main")]
            for ku in range(4):
                slot = self.wget(("wdn", l, ku * 16, u0, 256))
                for m2 in range(2):
                    for kt in range(16):
                        self.mm(ps2[m2][:, 0:TT], slot[:, kt, m2 * 128:(m2 + 1) * 128], self.hid[:, ku * 16 + kt, :],
                                ku == 0 and kt == 0, ku == 3 and kt == 15, inc=(kt == 15))
                if ws:
                    for m2 in range(2):
                        q = self.pss
                        c0 = (ku % 2) * 256 + m2 * 128
                        for kt in range(16):
                            self.mm(q[:, c0:c0 + NSMP], slot[:, kt, m2 * 128:(m2 + 1) * 128], self.hids[:, ku * 16 + kt, 0:NSMP],
                                    kt == 0, kt == 15)
                        m = u0 // 128 + m2
                        self.tt("dve", self.xs[:, m, 0:NSMP], self.xs[:, m, 0:NSMP], q[:, c0:c0 + NSMP], ALU.add)
            for m2 in range(2):
                m = u0 // 128 + m2
                self.tt("dve", self.xT[:, m, :], self.xT[:, m, :], ps2[m2][:, 0:TT], ALU.add)

    def run(self):
        self.load_consts()
        gi = 0
        for t in range(self.ntile):
            for kt in range(16):
                self.dma("sp", self.xT.t[:, kt, :], self.d_xp[t, :, kt, :], writes=[self.xT[:, kt, :]])
            ws = (t == 0) and not DBG_NOSAMPLE
            if ws:
                self.dma("sp", self.xs.t[:, :, :], self.d_xs[:, :, :], writes=[self.xs[:, :, :]])
            for l in range(self.depth):
                self.load_prm(l, gi)
                gi += 1
                if DBG_STOP < 1: break
                self.norm(self.xT, 16, TT, P_NMIX, self.hT, 1.0 / D)
                if ws:
                    self.norm(self.xs, 16, NSMP, P_NMIX, self.hTs, 1.0 / D)
                if DBG_STOP < 2: break
                self.gla(t, l, ws and DBG_SSTOP >= 2)
                if DBG_STOP < 3: break
                self.conv(t, l, ws and DBG_SSTOP >= 3)
                if DBG_STOP < 4: break
                g1 = self.merge_gen(t, l, ws, [0, 1])
                self.swa(t, l, ws, bg=(None if ws else g1))
                for _ in g1:
                    pass
                for _ in self.merge_gen(t, l, ws, [2]):
                    pass
                if DBG_STOP < 6: break
                self.wout(t, l, ws and DBG_SSTOP >= 6)
                self.norm(self.xT, 16, TT, P_NMLP, self.hT, 1.0 / D)
                if ws:
                    self.norm(self.xs, 16, NSMP, P_NMLP, self.hTs, 1.0 / D)
                if DBG_STOP < 7: break
                self.mlp(t, l, ws and DBG_SSTOP >= 7)
            if DBG_STOP < 8: continue
            def fin(kt, t=t):
                yo = self.yout[kt % 2]
                self.stt("dve", yo[:, :], self.xT[:, kt, :], self.prm[:, P_NFIN + kt:P_NFIN + kt + 1], self.rs[:, :],
                         ALU.mult, ALU.mult)
                self.dma("sp", self.o_yp[t, :, kt, :], yo.t[:, :], reads=[yo[:, :]])
            self.norm(self.xT, 16, TT, P_NFIN, fin, 1.0 / D)
            if ws:
                sm = self.smisc
                def fins(kt):
                    self.stt("dve", sm[:, 64 + 4 * kt:64 + 4 * kt + NSMP], self.xs[:, kt, 0:NSMP],
                             self.prm[:, P_NFIN + kt:P_NFIN + kt + 1], self.rs[:, 0:NSMP], ALU.mult, ALU.mult)
                self.norm(self.xs, 16, NSMP, P_NFIN, fins, 1.0 / D)
                self.dma("sp", self.o_ys.rearrange("p k b -> p (k b)"), sm.t[:, 64:128], reads=[sm[:, 64:128]])


class _View3:
    def __init__(self, buf, base, nm, n):
        self.buf, self.base, self.nm, self.n = buf, base, nm, n

    def __getitem__(self, idx):
        ps_, m, js = idx
        a = 0 if js.start is None else js.start
        b = self.n if js.stop is None else js.stop
        c0 = self.base + m * self.n
        return self.buf[ps_, c0 + a:c0 + b]


def build_program(depth=DEPTH, ntile=SEQ // TT):
    nc1 = bass.Bass("TRN2", target_bir_lowering=False)
    k1 = Kern(nc1, depth, ntile, None)
    k1.run()
    seq = k1.wrec
    nc = bass.Bass("TRN2", target_bir_lowering=False)
    k = Kern(nc, depth, ntile, seq)
    k.run()
    assert k.widx == len(seq)
    k.S.emit()
    return nc, k


def _consts():
    c = np.zeros((128, NCST), np.float32)
    c[:, K_ONES:K_ONES + 128] = 1.0
    c[:, K_ID:K_ID + 128] = np.eye(128, dtype=np.float32)
    s = np.arange(128)[:, None]
    q = np.arange(128)[None, :]
    same = (s // 64) == (q // 64)
    c[:, K_TRI:K_TRI + 128] = np.where(same & (s <= q), -1.0 / 16.0, 0.0)
    c[:, K_GT:K_GT + 128] = np.where(same & (s > q), -1.0 / 16.0, 0.0)
    c[:, K_BD:K_BD + 128] = np.where(same & (s <= q), 1.0, 0.0)
    tq = np.arange(128)[:, None]
    j = np.arange(256)[None, :]
    dist = tq + 128 - j
    dp = np.where((dist >= 0) & (dist < 128), dist, 1.0e6).astype(np.float32)
    c[:, K_DP:K_DP + 256] = dp
    dpf = dp.copy()
    dpf[:, 0:128] = 1.0e6
    c[:, K_DPF:K_DPF + 256] = dpf
    for b in range(4):
        c[b, K_SEL + b * 128:K_SEL + (b + 1) * 128] = 1.0
    jj = np.arange(128)
    for g in range(4):
        for kvh in range(4):
            c[g, K_BS + kvh * 128:K_BS + (kvh + 1) * 128] = -SLOPES[kvh * 4 + g] * (127 - jj)
    return c


def _prm(b_lr, gla_norm, conv_w, attn_sinks, norm_mix, norm_mlp, norm_final, L):
    p = np.zeros((L, 128, NPRM), np.float32)
    for l in range(L):
        p[l, :, P_NMIX:P_NMIX + 16] = norm_mix[l].reshape(16, 128).T
        p[l, :, P_NMLP:P_NMLP + 16] = norm_mlp[l].reshape(16, 128).T
        p[l, :, P_BLR:P_BLR + 4] = b_lr[l].reshape(4, 128).T
        p[l, :, P_GN:P_GN + 2] = gla_norm[l].reshape(2, 128).T
        p[l, :, P_CW:P_CW + 24] = conv_w[l].reshape(3, 8, 128).transpose(2, 0, 1).reshape(128, 24)
        p[l, :, P_SINK:P_SINK + 16] = attn_sinks[l][None, :]
        p[l, 0:4, P_SINKS:P_SINKS + 4] = attn_sinks[l].reshape(4, 4).T
        p[l, :, P_BLRBC:P_BLRBC + 512] = b_lr[l][None, :]
        p[l, :, P_NFIN:P_NFIN + 16] = norm_final.reshape(16, 128).T
    return p


_CACHE = {}


def kernel(x_prompt, x_sample, state_gla, state_conv, cache_k, cache_v, w_in, w_lr, b_lr, gla_norm, conv_w,
           attn_sinks, w_branch, w_out, norm_mix, norm_mlp, w_up, w_down, norm_final):
    f = lambda a: np.ascontiguousarray(np.asarray(a, dtype=np.float32))
    x_prompt, x_sample, state_gla, state_conv, cache_k, cache_v = map(f, (x_prompt, x_sample, state_gla, state_conv, cache_k, cache_v))
    w_in, w_lr, w_branch, w_out, w_up, w_down = map(f, (w_in, w_lr, w_branch, w_out, w_up, w_down))
    L = w_in.shape[0]
    B, SQ, _ = x_prompt.shape
    ntile = SQ // TT
    key = (L, ntile)
    if key not in _CACHE:
        _CACHE[key] = build_program(L, ntile)
    nc, _k = _CACHE[key]
    cst = _consts()
    prm = _prm(f(b_lr), f(gla_norm), f(conv_w), f(attn_sinks), f(norm_mix), f(norm_mlp), f(norm_final), L)
    ncores = 8
    in_maps = []
    xp_l = {}
    REAL = [0, 1, 4, 5]
    xzero = np.zeros((ntile, 128, 16, TT), np.float32)
    for c in range(ncores):
        if c in REAL:
            s = REAL.index(c)
            xp_l[s] = np.ascontiguousarray(x_prompt[s].reshape(ntile, TT, 16, 128).transpose(0, 3, 2, 1))
        else:
            s = -1
            xp_l[s] = xzero
        bs = slice(c * NSMP, (c + 1) * NSMP)
        ck = cache_k[:, bs].reshape(L, NSMP, 128, 256)
        in_maps.append({
            "xp": xp_l[s],
            "xs": np.ascontiguousarray(x_sample[bs, 0, :].reshape(NSMP, 16, 128).transpose(2, 1, 0)),
            "sg": np.ascontiguousarray(state_gla[:, bs]),
            "sc": np.ascontiguousarray(state_conv[:, bs].reshape(L, NSMP, 2, 8, 128).transpose(0, 4, 3, 1, 2)),
            "ck": np.ascontiguousarray(ck),
            "ckT": np.ascontiguousarray(cache_k[:, bs].transpose(0, 4, 1, 3, 2)),
            "cv": np.ascontiguousarray(cache_v[:, bs].reshape(L, NSMP, 128, 256)),
            "w_in": w_in, "w_lr": w_lr, "w_branch": w_branch, "w_out": w_out, "w_up": w_up, "w_down": w_down,
            "prm": prm, "cst": cst,
        })
    res = run_bass_kernel_spmd(nc, in_maps, core_ids=list(range(ncores)))
    R = res.results
    y_prompt = np.stack([R[REAL[s]]["o_yp"].transpose(0, 3, 2, 1).reshape(SQ, D) for s in range(B)])
    y_sample = np.concatenate([R[c]["o_ys"].transpose(2, 1, 0).reshape(NSMP, 1, D) for c in range(ncores)])
    sgp = np.stack([R[REAL[s]]["o_sgp"] for s in range(B)], axis=1)
    sgs = np.concatenate([R[c]["o_sgs"] for c in range(ncores)], axis=1)
    scp = np.stack([R[REAL[s]]["o_scp"].transpose(0, 3, 2, 1).reshape(L, 2, 1024) for s in range(B)], axis=1)
    scs = np.concatenate([R[c]["o_scs"].transpose(0, 3, 4, 2, 1).reshape(L, NSMP, 2, 1024) for c in range(ncores)], axis=1)
    ckp = np.stack([R[REAL[s]]["o_ckp"].reshape(L, 128, 4, 64) for s in range(B)], axis=1)
    cks = np.concatenate([R[c]["o_cks"].reshape(L, NSMP, 128, 4, 64) for c in range(ncores)], axis=1)
    cvp = np.stack([R[REAL[s]]["o_cvp"].reshape(L, 128, 4, 64) for s in range(B)], axis=1)
    cvs = np.concatenate([R[c]["o_cvs"].reshape(L, NSMP, 128, 4, 64) for c in range(ncores)], axis=1)
    out = (y_prompt, y_sample, sgp, sgs, scp, scs, ckp, cks, cvp, cvs)
    return tuple(np.ascontiguousarray(o, dtype=np.float32) for o in out)
```

```python
import math
import numpy as np
import concourse.bass as bass
import concourse.mybir as mybir
from concourse.bass_utils import run_bass_kernel_spmd

F32 = mybir.dt.float32
BF16 = mybir.dt.bfloat16
AF = mybir.ActivationFunctionType
ALU = mybir.AluOpType
AX = mybir.AxisListType

PAGE = 512
SEM_LIM = 20000
NDMA = 8
SB_BASE = 16896
SB_END = 229344

D = 2048
DEPTH = 4
SEQ = 2048
TT = 512
NSMP = 4
C_Q, C_K, C_V, C_GR, C_GLR, C_CB, C_CC, C_CH, C_SQ, C_SK, C_SV, C_G, C_END = (
    0, 512, 1024, 2048, 3072, 3088, 4112, 5136, 6160, 7184, 7440, 7696, 13840)
EPS = 1e-6
SLOPES = [2.0 ** (-8.0 * (i + 1) / 16) for i in range(16)]
P_NMIX, P_NMLP, P_BLR, P_GN, P_CW, P_SINK, P_SINKS, P_BLRBC, P_NFIN, NPRM = 0, 16, 32, 36, 38, 62, 78, 82, 594, 610
K_ONES, K_ID, K_TRI, K_GT, K_BD, K_DP, K_DPF, K_SEL, K_BS, NCST = 0, 128, 256, 384, 512, 640, 896, 1152, 1664, 2176


import os
DBG_NOSAMPLE = bool(int(os.environ.get("K_NOSAMPLE", "0")))
DBG_STOP = int(os.environ.get("K_STOP", "99"))
DBG_SSTOP = int(os.environ.get("K_SSTOP", "99"))
DBG_SWS = int(os.environ.get("K_SWS", "99"))
DBG_M = int(os.environ.get("K_M", "255"))


class Acc:
    __slots__ = ("ap", "keys")

    def __init__(self, ap, keys):
        self.ap = ap
        self.keys = keys


class Buf:
    def __init__(self, nc, name, shape, dtype, offset):
        self.t = nc.alloc_sbuf_tensor_at(name, list(shape), dtype, offset=offset)
        self.base = offset
        self.esz = 4 if dtype == F32 else 2
        self.shape = list(shape)
        st = [1] * len(shape)
        for d in range(len(shape) - 2, 0, -1):
            st[d] = st[d + 1] * shape[d + 1]
        self.st = st
        n = 1
        for s in shape[1:]:
            n *= s
        self.nbytes = n * self.esz
        assert offset >= SB_BASE and offset + self.nbytes <= SB_END, (name, offset, self.nbytes)

    def keys_for(self, idx):
        if not isinstance(idx, tuple):
            idx = (idx,)
        lo = 0
        hi = 0
        for d in range(1, len(self.shape)):
            i = idx[d] if d < len(idx) else slice(None)
            if isinstance(i, slice):
                a = 0 if i.start is None else i.start
                b = self.shape[d] if i.stop is None else i.stop
                lo += a * self.st[d]
                hi += (b - 1) * self.st[d]
            else:
                lo += i * self.st[d]
                hi += i * self.st[d]
        lob = self.base + lo * self.esz
        hib = self.base + (hi + 1) * self.esz
        return range(lob // PAGE, (hib - 1) // PAGE + 1)

    def __getitem__(self, idx):
        return Acc(self.t[idx], self.keys_for(idx))

    def acc(self, ap, idx):
        return Acc(ap, self.keys_for(idx))


class PBuf:
    def __init__(self, nc, name, bank, dtype=F32):
        self.ncol = 512 if dtype == F32 else 1024
        self.t = nc.alloc_psum_tensor(name, [128, self.ncol], dtype)
        self.bank = bank
        self.q = self.ncol // 4

    def __getitem__(self, idx):
        cs = idx[1]
        a = 0 if cs.start is None else cs.start
        b = self.ncol if cs.stop is None else cs.stop
        keys = (("ps", self.bank),)
        return Acc(self.t[idx], keys)

    def acc(self, ap, a, b):
        keys = (("ps", self.bank),)
        return Acc(ap, keys)


class Sched:
    ENGS = ("pe", "act", "dve", "pool", "sp")

    def __init__(self, nc):
        self.nc = nc
        self.q = {e: [] for e in self.ENGS}
        self.semh = {}
        self.seen = {e: {} for e in self.ENGS}
        self.state = {}
        self.dma_val = {q: [0] * NDMA for q in ("sp", "pool")}
        self.dma_rr = {"sp": 0, "pool": 0}
        self.n_ops = 0
        for qn in ("sp", "pool"):
            for i in range(NDMA):
                self.semh[("dma" + qn, i)] = nc.alloc_semaphore(name=f"sd{qn}{i}")

    def _sem(self, sk):
        h = self.semh.get(sk)
        if h is None:
            h = self.nc.alloc_semaphore(name=f"s_{sk[0]}_{sk[1]}")
            self.semh[sk] = h
        return h

    def _deps(self, eng, reads, writes):
        deps = {}
        st = self.state
        for a in reads:
            for k in a.keys:
                s = st.get(k)
                if s is not None and s[0] is not None:
                    sk, v = s[0]
                    if v > deps.get(sk, -1):
                        deps[sk] = v
        for a in writes:
            for k in a.keys:
                s = st.get(k)
                if s is not None:
                    if s[0] is not None:
                        sk, v = s[0]
                        if v > deps.get(sk, -1):
                            deps[sk] = v
                    for sk, v in s[1].items():
                        if v > deps.get(sk, -1):
                            deps[sk] = v
        waits = []
        seen = self.seen[eng]
        for sk, v in deps.items():
            if eng == "pe" and sk == "pe":
                continue
            if seen.get(sk, -1) >= v:
                continue
            seen[sk] = v
            if isinstance(sk, str):
                self.q[sk][v][2] = True
            waits.append((sk, v))
        return waits

    def _record(self, reads, writes, sk, v):
        st = self.state
        for a in reads:
            for k in a.keys:
                s = st.get(k)
                if s is None:
                    st[k] = [None, {sk: v}]
                elif v > s[1].get(sk, -1):
                    s[1][sk] = v
        for a in writes:
            for k in a.keys:
                st[k] = [(sk, v), {}]

    def op(self, eng, fn, reads=(), writes=(), inc=True):
        waits = self._deps(eng, reads, writes)
        idx = len(self.q[eng])
        self.q[eng].append([waits, fn, False, None])
        self._record(reads, writes, eng, idx)
        self.n_ops += 1

    def dma(self, qeng, out, in_, reads=(), writes=(), **kw):
        i = self.dma_rr[qeng]
        self.dma_rr[qeng] = (i + 1) % NDMA
        sk = ("dma" + qeng, i)
        waits = self._deps(qeng, reads, writes)
        prev = self.dma_val[qeng][i]
        if prev > 0 and self.seen[qeng].get(sk, -1) < prev:
            self.seen[qeng][sk] = prev
            waits.append((sk, prev))
        v = prev + 16
        self.dma_val[qeng][i] = v

        def fn(e, out=out, in_=in_, kw=kw):
            return e.dma_start(out=out, in_=in_, **kw)

        self.q[qeng].append([waits, fn, False, sk])
        self._record(reads, writes, sk, v)
        self.n_ops += 1

    def emit(self):
        nc = self.nc
        val = {}
        for eng in self.ENGS:
            ep, cnt = 0, 0
            for idx, ent in enumerate(self.q[eng]):
                if ent[2]:
                    if cnt >= SEM_LIM:
                        ep += 1
                        cnt = 0
                    cnt += 1
                    val[(eng, idx)] = (self._sem((eng, ep)), cnt)
        fin = []
        for qn in ("sp", "pool"):
            for i in range(NDMA):
                if self.dma_val[qn][i] > 0:
                    fin.append((self._sem(("dma" + qn, i)), self.dma_val[qn][i]))
        q = self.q
        self.n_marked = len(val)

        def replay(eng, e):
            for idx, (waits, fn, marked, dsem) in enumerate(q[eng]):
                for (sk, v) in waits:
                    if isinstance(sk, str):
                        h, vv = val[(sk, v)]
                    else:
                        h, vv = self._sem(sk), v
                    e.wait_ge(h, vv)
                ins = fn(e)
                if dsem is not None:
                    ins.then_inc(self._sem(dsem), 16)
                elif marked:
                    ins.then_inc(val[(eng, idx)][0], 1)

        with nc.Block() as block:
            @block.tensor
            def _(e):
                replay("pe", e)

            @block.scalar
            def _(e):
                replay("act", e)

            @block.vector
            def _(e):
                replay("dve", e)

            @block.gpsimd
            def _(e):
                replay("pool", e)

            @block.sync
            def _(e):
                replay("sp", e)
                for (h, v) in fin:
                    e.wait_ge(h, v)


def _isacc(x):
    return isinstance(x, Acc)


class Kern:
    def __init__(self, nc, depth, ntile, wseq=None):
        self.nc = nc
        self.depth = depth
        self.ntile = ntile
        self.S = Sched(nc)
        self.wseq = wseq
        self.wrec = []
        self.widx = 0
        self.wissued = 0
        self.evi = 0
        self._dram()
        self._sbuf()

    def _dram(self):
        nc, L, NTI = self.nc, self.depth, self.ntile
        di = lambda n, s: nc.dram_tensor(n, s, F32, kind="ExternalInput").ap()
        do = lambda n, s: nc.dram_tensor(n, s, F32, kind="ExternalOutput").ap()
        self.d_xp = di("xp", [NTI, 128, 16, TT])
        self.d_xs = di("xs", [128, 16, NSMP])
        self.d_sg = di("sg", [L, NSMP, 4, 128, 256])
        self.d_sc = di("sc", [L, 128, 8, NSMP, 2])
        self.d_ck = di("ck", [L, NSMP, 128, 256])
        self.d_ckT = di("ckT", [L, 64, NSMP, 4, 128])
        self.d_cv = di("cv", [L, NSMP, 128, 256])
        self.d_win = di("w_in", [L, D, C_END])
        self.d_wlr = di("w_lr", [L, 16, 512])
        self.d_wbr = di("w_branch", [L, 3, 1024, D])
        self.d_wout = di("w_out", [L, D, D])
        self.d_wup = di("w_up", [L, D, 4 * D])
        self.d_wdn = di("w_down", [L, 4 * D, D])
        self.d_prm = di("prm", [L, 128, NPRM])
        self.d_cst = di("cst", [128, NCST])
        self.o_yp = do("o_yp", [NTI, 128, 16, TT])
        self.o_ys = do("o_ys", [128, 16, NSMP])
        self.o_sgp = do("o_sgp", [L, 4, 128, 256])
        self.o_sgs = do("o_sgs", [L, NSMP, 4, 128, 256])
        self.o_scp = do("o_scp", [L, 128, 8, 2])
        self.o_scs = do("o_scs", [L, 128, 8, NSMP, 2])
        self.o_ckp = do("o_ckp", [L, 128, 256])
        self.o_cks = do("o_cks", [L, NSMP, 128, 256])
        self.o_cvp = do("o_cvp", [L, 128, 256])
        self.o_cvs = do("o_cvs", [L, NSMP, 128, 256])

    def _sbuf(self):
        nc = self.nc
        off = [SB_BASE]

        def pa(name, shape, dt):
            b = Buf(nc, name, shape, dt, off[0])
            off[0] += (b.nbytes + PAGE - 1) // PAGE * PAGE
            return b

        L = self.depth
        self.cst = pa("cst", [128, NCST], F32)
        self.identb = pa("identb", [128, 128], BF16)
        self.cols = pa("cols", [128, 8], F32)
        self.xT = pa("xT", [128, 16, TT], F32)
        self.hT = pa("hT", [128, 16, TT], BF16)
        self.prm2 = [pa(f"prm{i}", [128, NPRM], F32) for i in range(2)]
        self.wlr = pa("wlr", [16, 512], F32)
        self.Sst = [pa(f"S{l}", [128, 4, 256], F32) for l in range(L)]
        self.uprev = [pa(f"up{l}", [128, 8, 2], F32) for l in range(L)]
        self.kprev = [pa(f"kp{l}", [128, 4, 128], BF16) for l in range(L)]
        self.vprev = [pa(f"vp{l}", [128, 512], BF16) for l in range(L)]
        self.wslot = [pa(f"ws{i}", [128, 16, 256], BF16) for i in range(3)]
        self.xs = pa("xs", [128, 16, NSMP], F32)
        self.hTs = pa("hTs", [128, 16, NSMP], BF16)
        self.oas = pa("oas", [128, 8, NSMP], BF16)
        self.obs = pa("obs", [128, 8, NSMP], BF16)
        self.ocs = pa("ocs", [128, 8, NSMP], BF16)
        self.ypres = pa("ypres", [128, 16, NSMP], BF16)
        self.hids = pa("hids", [128, 64, NSMP], BF16)
        self.smisc = pa("smisc", [128, 256], F32)
        A = off[0]
        self.A = A
        assert A + 86016 <= SB_END, (A, SB_END - A)

        def ar(name, shape, dt, o):
            return Buf(nc, name, shape, dt, A + o)

        self.out_a = ar("out_a", [128, 8, TT], BF16, 0)
        self.out_b = ar("out_b", [128, 8, TT], BF16, 8192)
        self.out_c = ar("out_c", [128, 8, TT], BF16, 16384)
        self.gr_s = ar("gr_s", [128, 8, TT], BF16, 24576)
        self.v_tok = ar("v_tok", [128, 4, 1024], BF16, 32768)
        self.qT = ar("qT", [128, 4, TT], BF16, 40960)
        self.kT = ar("kT", [128, 4, TT], BF16, 45056)
        self.kd = ar("kd", [128, 4, 512], BF16, 49152)
        self.sp_tok = ar("sp_tok", [128, 4, 512], F32, 53248)
        self.expnb = ar("expnb", [128, 4, TT], F32, 61440)
        self.expb = ar("expb", [128, 4, TT], F32, 69632)
        self.ekd = ar("ekd", [128, 4, 512], F32, 77824)
        self.oT = ar("oT", [128, 8, TT], F32, 53248)
        self.sqt = [ar(f"sqt{i}", [128, TT], F32, 77824 + 2048 * i) for i in range(2)]
        self.rs = ar("rs", [128, TT], F32, 81920)
        self.tmpf = ar("tmpf", [128, TT], F32, 83968)
        self.ATm = [ar(f"ATm{i}", [128, 128], BF16, 8192 + 256 * i) for i in range(4)]
        self.Sbf = ar("Sbf", [128, 4, 256], BF16, 9216)
        self.glr = ar("glr", [16, 512], F32, 11264)
        self.gcol = ar("gcol", [128, 4, 8], F32, 13312)
        self.ccs = ar("ccs", [128, 2, TT], F32, 32768)
        self.ubuf = ar("ubuf", [128, 2, TT + 2], F32, 36864)
        self.ct = [ar(f"ct{i}", [128, TT], F32, 41472 + 2048 * i) for i in range(2)]
        self.qsT = ar("qsT", [128, 8, TT], BF16, 24576)
        self.kdupT = ar("kdupT", [128, 4, 640], BF16, 32768)
        self.vdup = ar("vdup", [128, 5, 512], BF16, 37888)
        self.sb = [ar(f"sb{i}", [128, 256], F32, 43008 + 1024 * i) for i in range(2)]
        self.pex = [ar(f"pex{i}", [128, 256], F32, 45056 + 1024 * i) for i in range(2)]
        self.pn = [ar(f"pn{i}", [128, 256], BF16, 47104 + 512 * i) for i in range(4)]
        self.pTsb = [ar(f"pTsb{i}", [128, 256], BF16, 49152 + 512 * i) for i in range(4)]
        self.sc8 = [ar(f"sc8{i}", [128, 8], F32, 51200 + 512 * i) for i in range(4)]
        self.kout = ar("kout", [128, 256], F32, 43008)
        self.vout = ar("vout", [128, 256], F32, 44032)
        self.sig = [ar(f"sig{i}", [128, TT], F32, 24576 + 2048 * i) for i in range(2)]
        self.accm = [ar(f"accm{i}", [128, TT], F32, 28672 + 2048 * i) for i in range(2)]
        self.tmul = [ar(f"tmul{i}", [128, TT], F32, 32768 + 2048 * i) for i in range(2)]
        self.ypre = ar("ypre", [128, 16, TT], BF16, 65536)
        self.sigB = [ar(f"sigB{i}", [128, TT], F32, 53248 + 2048 * i) for i in range(2)]
        self.accmB = [ar(f"accmB{i}", [128, TT], F32, 57344 + 2048 * i) for i in range(2)]
        self.tmulB = [ar(f"tmulB{i}", [128, TT], F32, 61440 + 2048 * i) for i in range(2)]
        self.mcnt = 0
        self.hid = ar("hid", [128, 64, TT], BF16, 0)
        self.relu = [ar(f"relu{i}", [128, TT], F32, 65536 + 2048 * i) for i in range(2)]
        self.yout = [ar(f"yout{i}", [128, TT], F32, 69632 + 2048 * i) for i in range(2)]
        self.v_tok_s = ar("v_tok_s", [4, 1024], F32, 16384)
        self.stg = [ar(f"stg{i}", [128, 256], F32, 20480 + 1024 * i) for i in range(2)]
        self.gsm = ar("gsm", [128, 256], F32, 22528)
        self.grss = ar("grss", [128, 8, NSMP], BF16, 23552)
        self.csm = ar("csm", [128, 512], F32, 16384)
        self.kwinT = ar("kwinT", [64, NSMP, 4, 128], BF16, 53248)
        self.vwin = ar("vwin", [128, NSMP, 512], BF16, 57344)
        self.k_tok_s = ar("k_tok_s", [4, 256], F32, 61440)
        self.v_tok_s2 = ar("v_tok_s2", [4, 512], F32, 62464)
        self.v_row_s = ar("v_row_s", [4, 256], F32, 64512)
        self.sc_s = ar("sc_s", [4, 512], F32, 65536)
        self.e_s = ar("e_s", [4, 512], F32, 67584)
        self.p_s = ar("p_s", [4, 512], BF16, 69632)
        self.pT_s = ar("pT_s", [128, 16], BF16, 70656)
        self.cl_s = ar("cl_s", [4, 32], F32, 71168)
        self.qs64 = ar("qs64", [64, 16, NSMP], BF16, 71680)
        self.psm = [PBuf(nc, f"psm{i}", i) for i in range(4)]
        self.psa = [PBuf(nc, f"psa{i}", 4 + i) for i in range(2)]
        self.pst = PBuf(nc, "pst", 6, BF16)
        self.pss = PBuf(nc, "pss", 7)
        self.rr = {"main": 0, "aux": 0}

    def ps(self, pool):
        if pool == "main":
            p = self.psm[self.rr["main"] % 4]
            self.rr["main"] += 1
        else:
            p = self.psa[self.rr["aux"] % 2]
            self.rr["aux"] += 1
        return p

    def ev(self):
        self.evi += 1
        return "act" if self.evi % 2 else "dve"

    def mm(self, out, lhsT, rhs, start, stop, inc=None):
        self.S.op("pe", lambda e, o=out.ap, l=lhsT.ap, r=rhs.ap, s=start, p=stop: e.matmul(o, l, r, start=s, stop=p),
                  reads=[lhsT, rhs], writes=[out], inc=(stop if inc is None else inc))

    def transpose(self, out, in_, ident):
        self.S.op("pe", lambda e, o=out.ap, i=in_.ap, d=ident.ap: e.transpose(o, i, d),
                  reads=[in_, ident], writes=[out])

    def act(self, out, in_, func, bias=None, scale=None, accum=None):
        kw = {}
        reads = [in_]
        writes = [out]
        if bias is not None:
            kw["bias"] = bias.ap if _isacc(bias) else bias
            if _isacc(bias):
                reads.append(bias)
        if scale is not None:
            kw["scale"] = scale.ap if _isacc(scale) else scale
            if _isacc(scale):
                reads.append(scale)
        if accum is not None:
            kw["accum_out"] = accum.ap
            writes.append(accum)
        self.S.op("act", lambda e, o=out.ap, i=in_.ap, f=func, kw=kw: e.activation(o, i, f, **kw), reads, writes)

    def copy(self, eng, out, in_):
        if eng == "act":
            self.act(out, in_, AF.Copy)
        else:
            self.S.op(eng, lambda e, o=out.ap, i=in_.ap: e.tensor_copy(o, i), [in_], [out])

    def tt(self, eng, out, a, b, op):
        self.S.op(eng, lambda e, o=out.ap, x=a.ap, y=b.ap, op=op: e.tensor_tensor(o, x, y, op), [a, b], [out])

    def ts(self, eng, out, a, s1, op0, s2=None, op1=None):
        reads = [a]
        v1 = s1.ap if _isacc(s1) else s1
        if _isacc(s1):
            reads.append(s1)
        v2 = s2.ap if _isacc(s2) else s2
        if _isacc(s2):
            reads.append(s2)
        if op1 is None:
            self.S.op(eng, lambda e, o=out.ap, x=a.ap: e.tensor_scalar(o, x, v1, None, op0), reads, [out])
        else:
            self.S.op(eng, lambda e, o=out.ap, x=a.ap: e.tensor_scalar(o, x, v1, v2, op0, op1), reads, [out])

    def stt(self, eng, out, a, s, b, op0, op1):
        reads = [a, b]
        sv = s.ap if _isacc(s) else s
        if _isacc(s):
            reads.append(s)
        self.S.op(eng, lambda e, o=out.ap, x=a.ap, y=b.ap: e.scalar_tensor_tensor(o, x, sv, y, op0, op1), reads, [out])

    def red(self, out, a, op):
        self.S.op("dve", lambda e, o=out.ap, x=a.ap, op=op: e.tensor_reduce(o, x, AX.X, op), [a], [out])

    def recip(self, out, a):
        self.S.op("dve", lambda e, o=out.ap, x=a.ap: e.reciprocal(o, x), [a], [out])

    def memset(self, eng, out, val):
        self.S.op(eng, lambda e, o=out.ap, v=val: e.memset(o, v), [], [out])

    def dma(self, q, out, in_, reads=(), writes=(), **kw):
        self.S.dma(q, out, in_, reads=reads, writes=writes, **kw)

    def _unit_dmas(self, desc, slot):
        kind = desc[0]
        if kind == "win":
            _, l, c0, ncol = desc
            v = self.d_win[l].rearrange("(kt p) n -> p kt n", p=128)
            return [(slot.t[:, 0:16, 0:ncol], v[:, :, c0:c0 + ncol])]
        if kind == "windup":
            _, l, c0 = desc
            v = self.d_win[l].rearrange("(kt p) n -> p kt n", p=128)
            src = v[:, :, c0:c0 + 128].rearrange("p kt (a b) -> p kt a b", a=2)
            dv = slot.t[:, 0:16, 0:256].rearrange("p kt (a d b) -> p kt a d b", a=2, d=2)
            return [(dv[:, :, a, r, :], src[:, :, a, :]) for a in range(2) for r in range(2)]
        if kind == "wbr":
            _, l, n, c0, ncol = desc
            v = self.d_wbr[l, n].rearrange("(kt p) n -> p kt n", p=128)
            return [(slot.t[:, 0:8, 0:ncol], v[:, :, c0:c0 + ncol])]
        if kind == "wout":
            _, l, c0, ncol = desc
            v = self.d_wout[l].rearrange("(kt p) n -> p kt n", p=128)
            return [(slot.t[:, 0:16, 0:ncol], v[:, :, c0:c0 + ncol])]
        if kind == "wup":
            _, l, c0, ncol = desc
            v = self.d_wup[l].rearrange("(kt p) n -> p kt n", p=128)
            return [(slot.t[:, 0:16, 0:ncol], v[:, :, c0:c0 + ncol])]
        if kind == "wdn":
            _, l, k0, c0, ncol = desc
            v = self.d_wdn[l].rearrange("(kt p) n -> p kt n", p=128)
            return [(slot.t[:, 0:16, 0:ncol], v[:, k0:k0 + 16, c0:c0 + ncol])]
        raise ValueError(kind)

    def _issue(self, i):
        desc = self.wseq[i]
        slot = self.wslot[i % 3]
        for dst, src in self._unit_dmas(desc, slot):
            self.dma("pool", dst, src, writes=[slot[:, :, :]])

    def wget(self, desc):
        i = self.widx
        self.widx += 1
        if self.wseq is None:
            self.wrec.append(desc)
            slot = self.wslot[i % 3]
            for dst, src in self._unit_dmas(desc, slot):
                self.dma("pool", dst, src, writes=[slot[:, :, :]])
            return slot
        assert self.wseq[i] == desc, (i, self.wseq[i], desc)
        while self.wissued < min(len(self.wseq), i + 3):
            self._issue(self.wissued)
            self.wissued += 1
        return self.wslot[i % 3]

    def proj(self, mk, c0, c1, src, KT, N=TT, evF=None, evT=None, srcs=None, evFs=None, evTs=None, ntb=4):
        for u0 in range(c0, c1, 256):
            ncol = min(256, c1 - u0)
            slot = self.wget(mk(u0, ncol))
            if evF is not None:
                for m0 in range(0, ncol, 128):
                    mc = min(128, ncol - m0)
                    p = self.ps("main")
                    for kt in range(KT):
                        self.mm(p[0:mc, 0:N], slot[:, kt, m0:m0 + mc], src[:, kt, 0:N], kt == 0, kt == KT - 1)
                    evF(p, u0 + m0, mc)
                    if srcs is not None and evFs is not None:
                        q = self.pss
                        for kt in range(KT):
                            self.mm(q[0:mc, 0:NSMP], slot[:, kt, m0:m0 + mc], srcs[:, kt, 0:NSMP], kt == 0, kt == KT - 1)
                        evFs(q, u0 + m0, mc)
            if evT is not None:
                for tb in range(ntb):
                    p = self.ps("main")
                    for kt in range(KT):
                        self.mm(p[:, 0:ncol], src[:, kt, tb * 128:(tb + 1) * 128], slot[:, kt, 0:ncol], kt == 0, kt == KT - 1)
                    evT(p, tb, u0, ncol)
            if srcs is not None and evTs is not None:
                q = self.pss
                for kt in range(KT):
                    self.mm(q[0:NSMP, 0:ncol], srcs[:, kt, 0:NSMP], slot[:, kt, 0:ncol], kt == 0, kt == KT - 1)
                evTs(q, u0, ncol)

    def load_consts(self):
        self.dma("sp", self.cst.t[:, :], self.d_cst[:, :], writes=[self.cst[:, :]])
        self.copy("act", self.identb[:, :], self.cst[:, K_ID:K_ID + 128])
        self.memset("dve", self.cols[:, 0:1], EPS)
        self.ones = self.cst.acc(self.cst.t[:, K_ONES:K_ONES + 128], (slice(None), slice(K_ONES, K_ONES + 128)))

    def cs(self, c0, n, p=128):
        return self.cst[0:p, c0:c0 + n]

    def load_prm(self, l, gi):
        b = self.prm2[gi % 2]
        self.dma("sp", b.t[:, :], self.d_prm[l], writes=[b[:, :]])
        self.dma("sp", self.wlr.t[:, :], self.d_wlr[l], writes=[self.wlr[:, :]])
        self.prm = b

    def norm(self, x, nkt, N, gbase, out, inv_d):
        p = self.ps("aux")
        for kt in range(nkt):
            sq = self.sqt[kt % 2]
            self.act(sq[:, 0:N], x[:, kt, 0:N], AF.Square)
            self.mm(p[:, 0:N], self.ones, sq[:, 0:N], kt == 0, kt == nkt - 1)
        self.act(self.rs[:, 0:N], p[:, 0:N], AF.Sqrt, bias=self.cols[:, 0:1], scale=inv_d)
        self.recip(self.rs[:, 0:N], self.rs[:, 0:N])
        for kt in range(nkt):
            if callable(out):
                out(kt)
            else:
                self.stt("dve", out[:, kt, 0:N], x[:, kt, 0:N], self.prm[:, gbase + kt:gbase + kt + 1],
                         self.rs[:, 0:N], ALU.mult, ALU.mult)

    def gla(self, t, l, ws):
        prm = self.prm
        mk = lambda c, n: ("win", l, c, n)
        S_ = self.Sst[l]
        gs = self.gsm
        GLR_S, Q_S, K_S, A_S, OT_S, NB = 0, 8, 24, 40, 56, 88
        def ev_glr(p, c, mc):
            self.copy("act", self.glr[0:16, 0:TT], p[0:16, 0:TT])
        def ev_glr_s(q, c, mc):
            self.copy("act", gs[0:16, GLR_S:GLR_S + NSMP], q[0:16, 0:NSMP])
        self.proj(mk, C_GLR, C_GLR + 16, self.hT, 16, evF=ev_glr, srcs=self.hTs if ws else None, evFs=ev_glr_s)
        for tb in range(4):
            p = self.ps("aux")
            self.mm(p[:, 0:512], self.glr[0:16, tb * 128:(tb + 1) * 128], self.wlr[0:16, 0:512], True, True)
            self.tt("dve", self.sp_tok[:, tb, :], p[:, 0:512], prm[:, P_BLRBC:P_BLRBC + 512], ALU.add)
            self.act(self.sp_tok[:, tb, :], self.sp_tok[:, tb, :], AF.Exp, scale=-1.0)
            self.act(self.sp_tok[:, tb, :], self.sp_tok[:, tb, :], AF.Ln, bias=1.0)
        for tb in range(4):
            p = self.ps("aux")
            self.mm(p[:, 0:512], self.cs(K_GT, 128), self.sp_tok[:, tb, :], True, True)
            self.act(self.ekd[:, tb, :], p[:, 0:512], AF.Exp)
            for h in range(4):
                p = self.ps("aux")
                self.mm(p[:, 0:128], self.sp_tok[:, tb, h * 128:(h + 1) * 128], self.cs(K_TRI, 128), True, True)
                self.act(self.expb[:, h, tb * 128:(tb + 1) * 128], p[:, 0:128], AF.Exp)
                self.act(self.expnb[:, h, tb * 128:(tb + 1) * 128], p[:, 0:128], AF.Exp, scale=-1.0)
        for h in range(4):
            src = self.expb.acc(self.expb.t[:, h, 63:TT:64], (slice(None), h))
            self.copy("dve", self.gcol[:, h, 0:8], src)
        def ev_kF(p, c, mc):
            h = (c - C_K) // 128
            self.tt("dve", self.kT[:, h, :], p[:, 0:TT], self.expnb[:, h, :], ALU.mult)
        def ev_kT(p, tb, u0, ncol):
            cc = u0 - C_K
            self.tt("dve", self.kd[:, tb, cc:cc + ncol], p[:, 0:ncol], self.ekd[:, tb, cc:cc + ncol], ALU.mult)
        def ev_kF_s(q, c, mc):
            h = (c - C_K) // 128
            self.copy("act", gs[:, K_S + 4 * h:K_S + 4 * h + NSMP], q[:, 0:NSMP])
        self.proj(mk, C_K, C_K + 512, self.hT, 16, evF=ev_kF, evT=ev_kT, srcs=self.hTs if ws else None, evFs=ev_kF_s)
        def ev_q(p, c, mc):
            h = (c - C_Q) // 128
            self.stt("dve", self.qT[:, h, :], p[:, 0:TT], 128.0 ** -0.5, self.expb[:, h, :], ALU.mult, ALU.mult)
        def ev_q_s(q, c, mc):
            h = (c - C_Q) // 128
            self.act(gs[:, Q_S + 4 * h:Q_S + 4 * h + NSMP], q[:, 0:NSMP], AF.Copy, scale=128.0 ** -0.5)
        self.proj(mk, C_Q, C_Q + 512, self.hT, 16, evF=ev_q, srcs=self.hTs if ws else None, evFs=ev_q_s)
        def ev_vT(p, tb, u0, ncol):
            cc = u0 - C_V
            self.copy(self.ev(), self.v_tok[:, tb, cc:cc + ncol], p[:, 0:ncol])
        def ev_vT_s(q, u0, ncol):
            cc = u0 - C_V
            self.copy("act", self.v_tok_s[0:NSMP, cc:cc + ncol], q[0:NSMP, 0:ncol])
        self.proj(mk, C_V, C_V + 1024, self.hT, 16, evT=ev_vT, srcs=self.hTs if ws else None, evTs=ev_vT_s)
        def ev_gr(p, c, mc):
            m = (c - C_GR) // 128
            self.act(self.gr_s[:, m, :], p[:, 0:TT], AF.Silu)
        def ev_gr_s(q, c, mc):
            m = (c - C_GR) // 128
            self.act(self.grss[:, m, 0:NSMP], q[:, 0:NSMP], AF.Silu)
        self.proj(mk, C_GR, C_GR + 1024, self.hT, 16, evF=ev_gr, srcs=self.hTs if ws else None, evFs=ev_gr_s)
        if t == 0:
            self.memset("dve", S_[:, :, :], 0.0)
        self.copy("act", self.Sbf[:, :, :], S_[:, :, :])
        po = [self.psm[0], self.psm[1]]
        for tb in range(4):
            for h in range(4):
                pA = self.ps("aux")
                blk = slice(tb * 128, (tb + 1) * 128)
                self.mm(pA[:, 0:128], self.kT[:, h, blk], self.qT[:, h, blk], True, True)
                self.tt("dve", self.ATm[h][:, :], pA[:, 0:128], self.cs(K_BD, 128), ALU.mult)
            for ch in range(2):
                rsl = slice(ch * 64, ch * 64 + 64)
                tok = slice(tb * 128 + ch * 64, tb * 128 + ch * 64 + 64)
                ci = tb * 2 + ch
                for h in range(4):
                    pb = po[h // 2]
                    for half in range(2):
                        o0 = (h % 2) * 256 + half * 128 + ch * 64
                        self.mm(pb[:, o0:o0 + 64], self.v_tok[rsl, tb, h * 256 + half * 128:h * 256 + half * 128 + 128],
                                self.ATm[h][rsl, ch * 64:ch * 64 + 64], True, False)
                        self.mm(pb[:, o0:o0 + 64], self.Sbf[:, h, half * 128:(half + 1) * 128],
                                self.qT[:, h, tok], False, True)
                    pU = self.psm[2 + (h % 2)]
                    self.mm(pU[:, 0:256], self.kd[rsl, tb, h * 128:(h + 1) * 128],
                            self.v_tok[rsl, tb, h * 256:(h + 1) * 256], True, True)
                    self.stt("dve", S_[:, h, :], S_[:, h, :], self.gcol[:, h, ci:ci + 1], pU[:, 0:256], ALU.mult, ALU.add)
                    self.copy("act", self.Sbf[:, h, :], S_[:, h, :])
            for h in range(4):
                pb = po[h // 2]
                for half in range(2):
                    o0 = (h % 2) * 256 + half * 128
                    self.copy(self.ev(), self.oT[:, h * 2 + half, tb * 128:(tb + 1) * 128], pb[:, o0:o0 + 128])
        if t == self.ntile - 1:
            self.dma("sp", self.o_sgp[l].rearrange("h k v -> k h v"), S_.t[:, :, :], reads=[S_[:, :, :]])
        self.gla_norm(self.oT, TT, self.gr_s, self.out_a)
        if ws:
            self.ts("dve", gs[:, NB:NB + 4], prm[:, P_BLR:P_BLR + 4], -1.0, ALU.mult)
            for h in range(4):
                q = self.pss
                self.mm(q[:, 0:NSMP], self.wlr[0:16, h * 128:(h + 1) * 128], gs[0:16, GLR_S:GLR_S + NSMP], True, True)
                a_h = gs[:, A_S + 4 * h:A_S + 4 * h + NSMP]
                self.act(a_h, q[:, 0:NSMP], AF.Exp, bias=gs[:, NB + h:NB + h + 1], scale=-1.0)
                self.act(a_h, a_h, AF.Ln, bias=1.0)
                self.act(a_h, a_h, AF.Exp, scale=-1.0 / 16.0)
            n = 0
            for b in range(NSMP):
                for h in range(4):
                    st = self.stg[n % 2]
                    n += 1
                    self.dma("sp", st.t[:, :], self.d_sg[l, b, h], writes=[st[:, :]])
                    q = self.ps("aux")
                    self.mm(q[:, 0:256], self.cst[0:NSMP, K_SEL + b * 128:K_SEL + (b + 1) * 128],
                            self.v_tok_s[0:NSMP, h * 256:(h + 1) * 256], True, True)
                    self.ts("dve", st[:, :], st[:, :], gs[:, A_S + 4 * h + b:A_S + 4 * h + b + 1], ALU.mult)
                    self.stt("dve", st[:, :], q[:, 0:256], gs[:, K_S + 4 * h + b:K_S + 4 * h + b + 1], st[:, :],
                             ALU.mult, ALU.add)
                    self.dma("sp", self.o_sgs[l, b, h], st.t[:, :], reads=[st[:, :]])
                    q2 = self.pss
                    for half in range(2):
                        self.mm(q2[:, half:half + 1], st[:, half * 128:(half + 1) * 128],
                                gs[:, Q_S + 4 * h + b:Q_S + 4 * h + b + 1], True, True)
                    oslot = gs.acc(gs.t[:, OT_S + (h * 2) * 4 + b:OT_S + (h * 2 + 2) * 4 + b:4],
                                   (slice(None), slice(OT_S, OT_S + 32)))
                    self.copy("dve", oslot, q2[:, 0:2])
            oTs = _View3(gs, OT_S, 8, NSMP)
            self.gla_norm(oTs, NSMP, self.grss, self.oas)

    def gla_norm(self, oT, N, gr, out):
        prm = self.prm
        for h in range(4):
            p = self.ps("aux")
            for half in range(2):
                sq = self.sqt[half]
                self.act(sq[:, 0:N], oT[:, h * 2 + half, 0:N], AF.Square)
                self.mm(p[:, 0:N], self.ones, sq[:, 0:N], half == 0, half == 1)
            self.act(self.rs[:, 0:N], p[:, 0:N], AF.Sqrt, bias=self.cols[:, 0:1], scale=1.0 / 256.0)
            self.recip(self.rs[:, 0:N], self.rs[:, 0:N])
            for half in range(2):
                m = h * 2 + half
                self.stt("dve", self.tmpf[:, 0:N], oT[:, m, 0:N], prm[:, P_GN + half:P_GN + half + 1],
                         self.rs[:, 0:N], ALU.mult, ALU.mult)
                self.tt("dve", out[:, m, 0:N], self.tmpf[:, 0:N], gr[:, m, 0:N], ALU.mult)

    def sample_window_loads(self, l):
        lst = [lambda: self.dma("pool", self.kwinT.t[:, :, :, 0:127], self.d_ckT[l][:, :, :, 1:128],
                                writes=[self.kwinT[:, :, :, :]])]
        vsrc = self.d_cv[l][:, 1:128, :].rearrange("b j (k d) -> j b k d", k=4)
        vdst = self.vwin.t[0:127, :, :].rearrange("j b (k r d) -> j b k r d", k=4, r=2)
        for b in range(NSMP):
            for r in range(2):
                lst.append(lambda b=b, r=r: self.dma("pool", vdst[:, b, :, r, :], vsrc[:, b, :, :],
                                                     writes=[self.vwin[:, b, :]]))
        return lst

    def conv(self, t, l, ws):
        prm = self.prm
        mk = lambda c, n: ("win", l, c, n)
        up = self.uprev[l]
        cm = self.csm
        CC_S, CH_S, PRV, USM, T_S = 0, 32, 64, 128, 192
        if t == 0:
            self.memset("dve", up[:, :, :], 0.0)
        if ws:
            self.dma("sp", cm.t[:, PRV:PRV + 64], self.d_sc[l].rearrange("p k b j -> p (k b j)"),
                     writes=[cm[:, PRV:PRV + 64]])
        pend = self.sample_window_loads(l) if ws else []
        for pr_ in range(4):
            for _ in range(3):
                if pend:
                    pend.pop(0)()
            def ev_cc(p, c, mc):
                m = (c - C_CC) // 128
                self.copy("act", self.ccs[:, m % 2, :], p[:, 0:TT])
            def ev_cc_s(q, c, mc):
                m = (c - C_CC) // 128
                self.copy("act", cm[:, CC_S + 4 * m:CC_S + 4 * m + NSMP], q[:, 0:NSMP])
            self.proj(mk, C_CC + pr_ * 256, C_CC + pr_ * 256 + 256, self.hT, 16, evF=ev_cc,
                      srcs=self.hTs if ws else None, evFs=ev_cc_s)
            def ev_ch(p, c, mc):
                m = (c - C_CH) // 128
                self.copy("act", self.ubuf[:, m % 2, 0:2], up[:, m, 0:2])
                self.tt("dve", self.ubuf[:, m % 2, 2:TT + 2], p[:, 0:TT], self.ccs[:, m % 2, :], ALU.mult)
                self.copy("act", up[:, m, 0:2], self.ubuf[:, m % 2, TT:TT + 2])
            def ev_ch_s(q, c, mc):
                m = (c - C_CH) // 128
                self.tt("dve", cm[:, USM + 4 * m:USM + 4 * m + NSMP], q[:, 0:NSMP],
                        cm[:, CC_S + 4 * m:CC_S + 4 * m + NSMP], ALU.mult)
            self.proj(mk, C_CH + pr_ * 256, C_CH + pr_ * 256 + 256, self.hT, 16, evF=ev_ch,
                      srcs=self.hTs if ws else None, evFs=ev_ch_s)
            def ev_cb(p, c, mc):
                m = (c - C_CB) // 128
                u = self.ubuf
                w = lambda j: prm[:, P_CW + j * 8 + m:P_CW + j * 8 + m + 1]
                c0, c1 = self.ct
                self.ts("dve", c0[:, :], u[:, m % 2, 2:TT + 2], w(2), ALU.mult)
                self.stt("dve", c1[:, :], u[:, m % 2, 1:TT + 1], w(1), c0[:, :], ALU.mult, ALU.add)
                self.stt("dve", c0[:, :], u[:, m % 2, 0:TT], w(0), c1[:, :], ALU.mult, ALU.add)
                self.tt("dve", self.out_b[:, m, :], p[:, 0:TT], c0[:, :], ALU.mult)
            def ev_cb_s(q, c, mc):
                m = (c - C_CB) // 128
                w = lambda j: prm[:, P_CW + j * 8 + m:P_CW + j * 8 + m + 1]
                pv = lambda j: cm.acc(cm.t[:, PRV + m * 8 + j:PRV + m * 8 + 8 + j:2],
                                      (slice(None), slice(PRV + m * 8, PRV + m * 8 + 8)))
                us = cm[:, USM + 4 * m:USM + 4 * m + NSMP]
                ta = cm[:, T_S:T_S + NSMP]
                tb_ = cm[:, T_S + 4:T_S + 4 + NSMP]
                self.ts("dve", ta, us, w(2), ALU.mult)
                self.stt("dve", tb_, pv(1), w(1), ta, ALU.mult, ALU.add)
                self.stt("dve", ta, pv(0), w(0), tb_, ALU.mult, ALU.add)
                self.tt("dve", self.obs[:, m, 0:NSMP], q[:, 0:NSMP], ta, ALU.mult)
                o0 = cm.acc(cm.t[:, 256 + m * 8:256 + m * 8 + 8:2], (slice(None), slice(256 + m * 8, 256 + m * 8 + 8)))
                o1 = cm.acc(cm.t[:, 256 + m * 8 + 1:256 + m * 8 + 9:2], (slice(None), slice(256 + m * 8, 256 + m * 8 + 8)))
                self.copy("dve", o0, pv(1))
                self.copy("dve", o1, us)
            self.proj(mk, C_CB + pr_ * 256, C_CB + pr_ * 256 + 256, self.hT, 16, evF=ev_cb,
                      srcs=self.hTs if ws else None, evFs=ev_cb_s)
        if t == self.ntile - 1:
            self.dma("sp", self.o_scp[l], up.t[:, :, :], reads=[up[:, :, :]])
        if ws:
            self.dma("sp", self.o_scs[l].rearrange("p k b j -> p (k b j)"), cm.t[:, 256:320], reads=[cm[:, 256:320]])

    def swa(self, t, l, ws, bg=None):
        prm = self.prm
        last = (t == self.ntile - 1)
        kp, vp = self.kprev[l], self.vprev[l]
        if t == 0:
            self.memset("dve", kp[:, :, :], 0.0)
            self.memset("dve", vp[:, :], 0.0)
        ws0 = ws
        ws = ws0 and DBG_SWS >= 2
        if ws0 and DBG_SWS >= 1:
            self.dma("sp", self.o_cks[l][:, 0:127, :], self.d_ck[l][:, 1:128, :])
            self.dma("sp", self.o_cvs[l][:, 0:127, :], self.d_cv[l][:, 1:128, :])
        def ev_sq(p, c, mc):
            m = (c - C_SQ) // 128
            self.act(self.qsT[:, m, :], p[:, 0:TT], AF.Copy, scale=0.125)
        for u0 in range(C_SQ, C_SQ + 1024, 256):
            slot = self.wget(("win", l, u0, 256))
            for m0 in range(0, 256, 128):
                p = self.ps("main")
                for kt in range(16):
                    self.mm(p[:, 0:TT], slot[:, kt, m0:m0 + 128], self.hT[:, kt, :], kt == 0, kt == 15)
                ev_sq(p, u0 + m0, 128)
            if ws and (DBG_M & 1):
                for hh in range(4):
                    hq = (u0 - C_SQ) // 64 + hh
                    q = self.pss
                    for kt in range(16):
                        self.mm(q[0:64, 0:NSMP], slot[:, kt, hh * 64:(hh + 1) * 64], self.hTs[:, kt, 0:NSMP], kt == 0, kt == 15)
                    self.act(self.qs64[0:64, hq, 0:NSMP], q[0:64, 0:NSMP], AF.Copy, scale=0.125)
        self.copy("act", self.kdupT[:, :, 0:128], kp[:, :, :])
        for pair in range(2):
            slot = self.wget(("windup", l, C_SK + pair * 128))
            for kvl in range(2):
                kvh = pair * 2 + kvl
                p = self.ps("main")
                for kt in range(16):
                    self.mm(p[:, 0:TT], slot[:, kt, kvl * 128:(kvl + 1) * 128], self.hT[:, kt, :], kt == 0, kt == 15)
                self.copy(self.ev(), self.kdupT[:, kvh, 128:640], p[:, 0:TT])
                if ws and (DBG_M & 2):
                    q = self.pss
                    for kt in range(16):
                        self.mm(q[0:64, 0:NSMP], slot[:, kt, kvl * 128:kvl * 128 + 64], self.hTs[:, kt, 0:NSMP], kt == 0, kt == 15)
                    dst = self.kwinT.acc(self.kwinT.t[0:64, :, kvh, 127], (slice(None),))
                    self.copy("act", dst, q[0:64, 0:NSMP])
            if last:
                p = self.ps("main")
                for kt in range(16):
                    self.mm(p[:, 0:256], self.hT[:, kt, 384:512], slot[:, kt, 0:256], kt == 0, kt == 15)
                src = p.acc(p.t[:, 0:256].rearrange("p (a r d) -> p a r d", a=2, r=2)[:, :, 0, :], 0, 256)
                dst = self.kout.acc(self.kout.t[:, pair * 128:(pair + 1) * 128].rearrange("p (a d) -> p a d", a=2),
                                    (slice(None), slice(pair * 128, pair * 128 + 128)))
                self.copy("dve", dst, src)
            if ws and (DBG_M & 4):
                q = self.pss
                for kt in range(16):
                    self.mm(q[0:NSMP, 0:256], self.hTs[:, kt, 0:NSMP], slot[:, kt, 0:256], kt == 0, kt == 15)
                src = q.acc(q.t[0:NSMP, 0:256].rearrange("p (a r d) -> p a r d", a=2, r=2)[:, :, 0, :], 0, 256)
                dst = self.k_tok_s.acc(self.k_tok_s.t[0:NSMP, pair * 128:(pair + 1) * 128].rearrange("p (a d) -> p a d", a=2),
                                       (slice(None), slice(pair * 128, pair * 128 + 128)))
                self.copy("dve", dst, src)
        if last:
            self.dma("sp", self.o_ckp[l], self.kout.t[:, :], reads=[self.kout[:, :]])
        if ws and (DBG_M & 4):
            self.dma("sp", self.o_cks[l][:, 127, :], self.k_tok_s.t[0:NSMP, :], reads=[self.k_tok_s[:, :]])
        self.copy("act", self.vdup[:, 0, :], vp[:, :])
        for pair in range(2):
            slot = self.wget(("windup", l, C_SV + pair * 128))
            for tb in range(4):
                p = self.ps("main")
                for kt in range(16):
                    self.mm(p[:, 0:256], self.hT[:, kt, tb * 128:(tb + 1) * 128], slot[:, kt, 0:256], kt == 0, kt == 15)
                self.copy(self.ev(), self.vdup[:, tb + 1, pair * 256:(pair + 1) * 256], p[:, 0:256])
                if last and tb == 3:
                    src = p.acc(p.t[:, 0:256].rearrange("p (a r d) -> p a r d", a=2, r=2)[:, :, 0, :], 0, 256)
                    dst = self.vout.acc(self.vout.t[:, pair * 128:(pair + 1) * 128].rearrange("p (a d) -> p a d", a=2),
                                        (slice(None), slice(pair * 128, pair * 128 + 128)))
                    self.copy("dve", dst, src)
            if ws and (DBG_M & 8):
                q = self.pss
                for kt in range(16):
                    self.mm(q[0:NSMP, 0:256], self.hTs[:, kt, 0:NSMP], slot[:, kt, 0:256], kt == 0, kt == 15)
                src = q.acc(q.t[0:NSMP, 0:256].rearrange("p (a r d) -> p a r d", a=2, r=2)[:, :, 0, :], 0, 256)
                dst = self.v_row_s.acc(self.v_row_s.t[0:NSMP, pair * 128:(pair + 1) * 128].rearrange("p (a d) -> p a d", a=2),
                                       (slice(None), slice(pair * 128, pair * 128 + 128)))
                self.copy("dve", dst, src)
        if last:
            self.dma("sp", self.o_cvp[l], self.vout.t[:, :], reads=[self.vout[:, :]])
        if ws and (DBG_M & 8):
            self.dma("sp", self.o_cvs[l][:, 127, :], self.v_row_s.t[0:NSMP, :], reads=[self.v_row_s[:, :]])
        if ws and (DBG_M & 16):
            for b in range(NSMP):
                dstv = self.vwin.t[127:128, b, :].rearrange("p (k r d) -> p k r d", k=4, r=2)
                srcv = self.v_row_s.t[b:b + 1, :].rearrange("p (k d) -> p k d", k=4)
                for r in range(2):
                    self.dma("pool", dstv[:, :, r, :], srcv, reads=[self.v_row_s[:, :]], writes=[self.vwin[:, b, :]])
        items = [(tb, hq) for tb in range(4) for hq in range(16)]
        NI = len(items)

        def geo(i):
            tb, hq = items[i]
            kvh, m, half = hq // 4, hq // 2, hq % 2
            return tb, hq, kvh, m, slice(half * 64, half * 64 + 64), slice(tb * 128, (tb + 1) * 128)

        def st1(i):
            tb, hq, kvh, m, pr, blk = geo(i)
            p = self.psm[i % 2]
            self.mm(p[:, 0:256], self.qsT[pr, m, blk], self.kdupT[pr, kvh, tb * 128:tb * 128 + 256], True, True)

        def st2a(i):
            tb, hq, kvh, m, pr, blk = geo(i)
            first = (t == 0 and tb == 0)
            Dp = self.cs(K_DPF if first else K_DP, 256)
            p = self.psm[i % 2]
            sb, pex, sc = self.sb[i % 2], self.pex[i % 2], self.sc8[i % 4]
            sink = prm[:, P_SINK + hq:P_SINK + hq + 1]
            self.stt("dve", sb[:, :], Dp, -SLOPES[hq], p[:, 0:256], ALU.mult, ALU.add)
            self.red(sc[:, 0:1], sb[:, :], ALU.max)
            self.tt("dve", sc[:, 1:2], sc[:, 0:1], sink, ALU.max)
            self.ts("dve", sc[:, 2:3], sc[:, 1:2], -1.0, ALU.mult)
            self.act(pex[:, :], sb[:, :], AF.Exp, bias=sc[:, 2:3], accum=sc[:, 3:4])
            self.act(sc[:, 4:5], sink, AF.Exp, bias=sc[:, 2:3])

        def st2b(i):
            pex, pn, sc = self.pex[i % 2], self.pn[i % 4], self.sc8[i % 4]
            self.tt("dve", sc[:, 5:6], sc[:, 3:4], sc[:, 4:5], ALU.add)
            self.recip(sc[:, 6:7], sc[:, 5:6])
            if i % 2:
                self.act(pn[:, :], pex[:, :], AF.Copy, scale=sc[:, 6:7])
            else:
                self.ts("dve", pn[:, :], pex[:, :], sc[:, 6:7], ALU.mult)

        def st3(i):
            pn, pTs = self.pn[i % 4], self.pTsb[i % 4]
            for jb in range(2):
                self.transpose(self.pst[:, jb * 128:(jb + 1) * 128], pn[:, jb * 128:(jb + 1) * 128], self.identb[:, :])
            self.copy("act", pTs[:, :], self.pst[:, 0:256])

        def st4(i):
            tb, hq, kvh, m, pr, blk = geo(i)
            pTs = self.pTsb[i % 4]
            p2 = self.psa[i % 2]
            for jb in range(2):
                self.mm(p2[:, 0:128], self.vdup[:, tb + jb, kvh * 128:(kvh + 1) * 128], pTs[:, jb * 128:(jb + 1) * 128],
                        jb == 0, jb == 1)
            self.copy("dve", self.out_c[pr, m, blk], p2[pr, 0:128])

        for s_ in range(NI + 4):
            if s_ < NI:
                st1(s_)
            if 0 <= s_ - 1 < NI:
                st2a(s_ - 1)
            if 0 <= s_ - 2 < NI:
                st2b(s_ - 2)
            if 0 <= s_ - 3 < NI:
                st3(s_ - 3)
            if 0 <= s_ - 4 < NI:
                st4(s_ - 4)
            if bg is not None:
                next(bg, None)
        self.copy("act", kp[:, :, :], self.kdupT[:, :, 512:640])
        self.copy("act", vp[:, :], self.vdup[:, 4, :])
        if ws0 and DBG_SWS >= 3:
            cl = self.cl_s
            for b in range(NSMP):
                q = self.pss
                for kvh in range(4):
                    lhs = self.qs64.acc(self.qs64.t[0:64, kvh * 4:(kvh + 1) * 4, b], (slice(None),))
                    self.mm(q[0:4, kvh * 128:(kvh + 1) * 128], lhs, self.kwinT[0:64, b, kvh, :], True, True)
                self.tt("dve", self.sc_s[0:4, :], q[0:4, 0:512], self.cst[0:4, K_BS:K_BS + 512], ALU.add)
                v3 = self.sc_s.acc(self.sc_s.t[0:4, :].rearrange("p (k j) -> p k j", k=4), (slice(None),))
                self.red(cl[0:4, 0:4], v3, ALU.max)
                sk = prm[0:4, P_SINKS:P_SINKS + 4]
                self.tt("dve", cl[0:4, 4:8], cl[0:4, 0:4], sk, ALU.max)
                self.ts("dve", cl[0:4, 8:12], cl[0:4, 4:8], -1.0, ALU.mult)
                for kvh in range(4):
                    self.act(self.e_s[0:4, kvh * 128:(kvh + 1) * 128], self.sc_s[0:4, kvh * 128:(kvh + 1) * 128], AF.Exp,
                             bias=cl[0:4, 8 + kvh:9 + kvh], accum=cl[0:4, 12 + kvh:13 + kvh])
                self.tt("dve", cl[0:4, 16:20], sk, cl[0:4, 8:12], ALU.add)
                self.act(cl[0:4, 16:20], cl[0:4, 16:20], AF.Exp)
                self.tt("dve", cl[0:4, 20:24], cl[0:4, 12:16], cl[0:4, 16:20], ALU.add)
                self.recip(cl[0:4, 24:28], cl[0:4, 20:24])
                for kvh in range(4):
                    self.ts("dve", self.p_s[0:4, kvh * 128:(kvh + 1) * 128], self.e_s[0:4, kvh * 128:(kvh + 1) * 128],
                            cl[0:4, 24 + kvh:25 + kvh], ALU.mult)
                for kvh in range(4):
                    self.transpose(self.pst[:, kvh * 4:kvh * 4 + 4], self.p_s[0:4, kvh * 128:(kvh + 1) * 128],
                                   self.identb[0:4, 0:4])
                self.copy("act", self.pT_s[:, 0:16], self.pst[:, 0:16])
                for kvh in range(4):
                    q2 = self.ps("aux")
                    self.mm(q2[:, 0:4], self.vwin[:, b, kvh * 128:(kvh + 1) * 128], self.pT_s[:, kvh * 4:kvh * 4 + 4], True, True)
                    for g in range(4):
                        pr = slice((g % 2) * 64, (g % 2) * 64 + 64)
                        self.copy("dve", self.ocs[pr, kvh * 2 + g // 2, b:b + 1], q2[pr, g:g + 1])

    def merge_gen(self, t, l, ws, nlist):
        outs = [self.out_a, self.out_b, self.out_c]
        outs_s = [self.oas, self.obs, self.ocs]
        sm = self.smisc
        sigb, accb, tmb = (self.sig, self.accm, self.tmul) if ws else (self.sigB, self.accmB, self.tmulB)
        for mp in range(8):
            for n in nlist:
                gslot = self.wget(("win", l, C_G + n * 2048 + mp * 256, 256))
                for m2 in range(2):
                    p = self.psm[2 + self.mcnt % 2]
                    self.mcnt += 1
                    for kt in range(16):
                        self.mm(p[:, 0:TT], gslot[:, kt, m2 * 128:(m2 + 1) * 128], self.hT[:, kt, :], kt == 0, kt == 15)
                    yield
                    self.act(sigb[m2][:, :], p[:, 0:TT], AF.Sigmoid)
                    if ws:
                        q = self.pss
                        for kt in range(16):
                            self.mm(q[:, 0:NSMP], gslot[:, kt, m2 * 128:(m2 + 1) * 128], self.hTs[:, kt, 0:NSMP], kt == 0, kt == 15)
                        self.act(sm[:, m2 * 4:m2 * 4 + NSMP], q[:, 0:NSMP], AF.Sigmoid)
                bslot = self.wget(("wbr", l, n, mp * 256, 256))
                for m2 in range(2):
                    m = mp * 2 + m2
                    p = self.psm[2 + self.mcnt % 2]
                    self.mcnt += 1
                    for kt in range(8):
                        self.mm(p[:, 0:TT], bslot[:, kt, m2 * 128:(m2 + 1) * 128], outs[n][:, kt, :], kt == 0, kt == 7)
                    yield
                    ac = accb[m2]
                    if n == 0:
                        self.tt("dve", ac[:, :], sigb[m2][:, :], p[:, 0:TT], ALU.mult)
                    elif n == 1:
                        self.tt("dve", tmb[m2][:, :], sigb[m2][:, :], p[:, 0:TT], ALU.mult)
                        self.tt("dve", self.ypre[:, m, :], ac[:, :], tmb[m2][:, :], ALU.add)
                    else:
                        self.tt("dve", tmb[m2][:, :], sigb[m2][:, :], p[:, 0:TT], ALU.mult)
                        self.tt("dve", self.ypre[:, m, :], self.ypre[:, m, :], tmb[m2][:, :], ALU.add)
                    if ws:
                        q = self.pss
                        for kt in range(8):
                            self.mm(q[:, 0:NSMP], bslot[:, kt, m2 * 128:(m2 + 1) * 128], outs_s[n][:, kt, 0:NSMP], kt == 0, kt == 7)
                        acs = sm[:, 16 + m2 * 4:16 + m2 * 4 + NSMP]
                        tms = sm[:, 32 + m2 * 4:32 + m2 * 4 + NSMP]
                        sgs = sm[:, m2 * 4:m2 * 4 + NSMP]
                        if n == 0:
                            self.tt("dve", acs, sgs, q[:, 0:NSMP], ALU.mult)
                        elif n == 1:
                            self.tt("dve", tms, sgs, q[:, 0:NSMP], ALU.mult)
                            self.tt("dve", self.ypres[:, m, 0:NSMP], acs, tms, ALU.add)
                        else:
                            self.tt("dve", tms, sgs, q[:, 0:NSMP], ALU.mult)
                            self.tt("dve", self.ypres[:, m, 0:NSMP], self.ypres[:, m, 0:NSMP], tms, ALU.add)

    def wout(self, t, l, ws):
        def ev(p, c, mc):
            m = c // 128
            self.tt("dve", self.xT[:, m, :], self.xT[:, m, :], p[:, 0:TT], ALU.add)
        def ev_s(q, c, mc):
            m = c // 128
            self.tt("dve", self.xs[:, m, 0:NSMP], self.xs[:, m, 0:NSMP], q[:, 0:NSMP], ALU.add)
        self.proj(lambda c, n: ("wout", l, c, n), 0, D, self.ypre, 16, evF=ev,
                  srcs=self.ypres if ws else None, evFs=ev_s)

    def mlp(self, t, l, ws):
        sm = self.smisc
        def ev(p, c, mc):
            m = c // 128
            r = self.relu[m % 2]
            self.act(r[:, :], p[:, 0:TT], AF.Relu)
            self.tt("dve", self.hid[:, m, :], r[:, :], r[:, :], ALU.mult)
        def ev_s(q, c, mc):
            m = c // 128
            r = sm[:, 48:48 + NSMP]
            self.act(r, q[:, 0:NSMP], AF.Relu)
            self.tt("dve", self.hids[:, m, 0:NSMP], r, r, ALU.mult)
        self.proj(lambda c, n: ("wup", l, c, n), 0, 4 * D, self.hT, 16, evF=ev,
                  srcs=self.hTs if ws else None, evFs=ev_s)
        for u0 in range(0, D, 256):
            ps2 = [self.ps("main"), self.ps("main")]
            for ku in range(4):
                slot = self.wget(("wdn", l, ku * 16, u0, 256))
                for m2 in range(2):
                    for kt in range(16):
                        self.mm(ps2[m2][:, 0:TT], slot[:, kt, m2 * 128:(m2 + 1) * 128], self.hid[:, ku * 16 + kt, :],
                                ku == 0 and kt == 0, ku == 3 and kt == 15, inc=(kt == 15))
                if ws:
                    for m2 in range(2):
                        q = self.pss
                        c0 = (ku % 2) * 256 + m2 * 128
                        for kt in range(16):
                            self.mm(q[:, c0:c0 + NSMP], slot[:, kt, m2 * 128:(m2 + 1) * 128], self.hids[:, ku * 16 + kt, 0:NSMP],
                                    kt == 0, kt == 15)
                        m = u0 // 128 + m2
                        self.tt("dve", self.xs[:, m, 0:NSMP], self.xs[:, m, 0:NSMP], q[:, c0:c0 + NSMP], ALU.add)
            for m2 in range(2):
                m = u0 // 128 + m2
                self.tt("dve", self.xT[:, m, :], self.xT[:, m, :], ps2[m2][:, 0:TT], ALU.add)

    def run(self):
        self.load_consts()
        gi = 0
        for t in range(self.ntile):
            for kt in range(16):
                self.dma("sp", self.xT.t[:, kt, :], self.d_xp[t, :, kt, :], writes=[self.xT[:, kt, :]])
            ws = (t == 0) and not DBG_NOSAMPLE
            if ws:
                self.dma("sp", self.xs.t[:, :, :], self.d_xs[:, :, :], writes=[self.xs[:, :, :]])
            for l in range(self.depth):
                self.load_prm(l, gi)
                gi += 1
                if DBG_STOP < 1: break
                self.norm(self.xT, 16, TT, P_NMIX, self.hT, 1.0 / D)
                if ws:
                    self.norm(self.xs, 16, NSMP, P_NMIX, self.hTs, 1.0 / D)
                if DBG_STOP < 2: break
                self.gla(t, l, ws and DBG_SSTOP >= 2)
                if DBG_STOP < 3: break
                self.conv(t, l, ws and DBG_SSTOP >= 3)
                if DBG_STOP < 4: break
                g1 = self.merge_gen(t, l, ws, [0, 1])
                self.swa(t, l, ws, bg=(None if ws else g1))
                for _ in g1:
                    pass
                for _ in self.merge_gen(t, l, ws, [2]):
                    pass
                if DBG_STOP < 6: break
                self.wout(t, l, ws and DBG_SSTOP >= 6)
                self.norm(self.xT, 16, TT, P_NMLP, self.hT, 1.0 / D)
                if ws:
                    self.norm(self.xs, 16, NSMP, P_NMLP, self.hTs, 1.0 / D)
                if DBG_STOP < 7: break
                self.mlp(t, l, ws and DBG_SSTOP >= 7)
            if DBG_STOP < 8: continue
            def fin(kt, t=t):
                yo = self.yout[kt % 2]
                self.stt("dve", yo[:, :], self.xT[:, kt, :], self.prm[:, P_NFIN + kt:P_NFIN + kt + 1], self.rs[:, :],
                         ALU.mult, ALU.mult)
                self.dma("sp", self.o_yp[t, :, kt, :], yo.t[:, :], reads=[yo[:, :]])
            self.norm(self.xT, 16, TT, P_NFIN, fin, 1.0 / D)
            if ws:
                sm = self.smisc
                def fins(kt):
                    self.stt("dve", sm[:, 64 + 4 * kt:64 + 4 * kt + NSMP], self.xs[:, kt, 0:NSMP],
                             self.prm[:, P_NFIN + kt:P_NFIN + kt + 1], self.rs[:, 0:NSMP], ALU.mult, ALU.mult)
                self.norm(self.xs, 16, NSMP, P_NFIN, fins, 1.0 / D)
                self.dma("sp", self.o_ys.rearrange("p k b -> p (k b)"), sm.t[:, 64:128], reads=[sm[:, 64:128]])


class _View3:
    def __init__(self, buf, base, nm, n):
        self.buf, self.base, self.nm, self.n = buf, base, nm, n

    def __getitem__(self, idx):
        ps_, m, js = idx
        a = 0 if js.start is None else js.start
        b = self.n if js.stop is None else js.stop
        c0 = self.base + m * self.n
        return self.buf[ps_, c0 + a:c0 + b]


def build_program(depth=DEPTH, ntile=SEQ // TT):
    nc1 = bass.Bass("TRN2", target_bir_lowering=False)
    k1 = Kern(nc1, depth, ntile, None)
    k1.run()
    seq = k1.wrec
    nc = bass.Bass("TRN2", target_bir_lowering=False)
    k = Kern(nc, depth, ntile, seq)
    k.run()
    assert k.widx == len(seq)
    k.S.emit()
    return nc, k


def _consts():
    c = np.zeros((128, NCST), np.float32)
    c[:, K_ONES:K_ONES + 128] = 1.0
    c[:, K_ID:K_ID + 128] = np.eye(128, dtype=np.float32)
    s = np.arange(128)[:, None]
    q = np.arange(128)[None, :]
    same = (s // 64) == (q // 64)
    c[:, K_TRI:K_TRI + 128] = np.where(same & (s <= q), -1.0 / 16.0, 0.0)
    c[:, K_GT:K_GT + 128] = np.where(same & (s > q), -1.0 / 16.0, 0.0)
    c[:, K_BD:K_BD + 128] = np.where(same & (s <= q), 1.0, 0.0)
    tq = np.arange(128)[:, None]
    j = np.arange(256)[None, :]
    dist = tq + 128 - j
    dp = np.where((dist >= 0) & (dist < 128), dist, 1.0e6).astype(np.float32)
    c[:, K_DP:K_DP + 256] = dp
    dpf = dp.copy()
    dpf[:, 0:128] = 1.0e6
    c[:, K_DPF:K_DPF + 256] = dpf
    for b in range(4):
        c[b, K_SEL + b * 128:K_SEL + (b + 1) * 128] = 1.0
    jj = np.arange(128)
    for g in range(4):
        for kvh in range(4):
            c[g, K_BS + kvh * 128:K_BS + (kvh + 1) * 128] = -SLOPES[kvh * 4 + g] * (127 - jj)
    return c


def _prm(b_lr, gla_norm, conv_w, attn_sinks, norm_mix, norm_mlp, norm_final, L):
    p = np.zeros((L, 128, NPRM), np.float32)
    for l in range(L):
        p[l, :, P_NMIX:P_NMIX + 16] = norm_mix[l].reshape(16, 128).T
        p[l, :, P_NMLP:P_NMLP + 16] = norm_mlp[l].reshape(16, 128).T
        p[l, :, P_BLR:P_BLR + 4] = b_lr[l].reshape(4, 128).T
        p[l, :, P_GN:P_GN + 2] = gla_norm[l].reshape(2, 128).T
        p[l, :, P_CW:P_CW + 24] = conv_w[l].reshape(3, 8, 128).transpose(2, 0, 1).reshape(128, 24)
        p[l, :, P_SINK:P_SINK + 16] = attn_sinks[l][None, :]
        p[l, 0:4, P_SINKS:P_SINKS + 4] = attn_sinks[l].reshape(4, 4).T
        p[l, :, P_BLRBC:P_BLRBC + 512] = b_lr[l][None, :]
        p[l, :, P_NFIN:P_NFIN + 16] = norm_final.reshape(16, 128).T
    return p


_CACHE = {}


def kernel(x_prompt, x_sample, state_gla, state_conv, cache_k, cache_v, w_in, w_lr, b_lr, gla_norm, conv_w,
           attn_sinks, w_branch, w_out, norm_mix, norm_mlp, w_up, w_down, norm_final):
    f = lambda a: np.ascontiguousarray(np.asarray(a, dtype=np.float32))
    x_prompt, x_sample, state_gla, state_conv, cache_k, cache_v = map(f, (x_prompt, x_sample, state_gla, state_conv, cache_k, cache_v))
    w_in, w_lr, w_branch, w_out, w_up, w_down = map(f, (w_in, w_lr, w_branch, w_out, w_up, w_down))
    L = w_in.shape[0]
    B, SQ, _ = x_prompt.shape
    ntile = SQ // TT
    key = (L, ntile)
    if key not in _CACHE:
        _CACHE[key] = build_program(L, ntile)
    nc, _k = _CACHE[key]
    cst = _consts()
    prm = _prm(f(b_lr), f(gla_norm), f(conv_w), f(attn_sinks), f(norm_mix), f(norm_mlp), f(norm_final), L)
    ncores = 8
    in_maps = []
    xp_l = {}
    REAL = [0, 1, 4, 5]
    xzero = np.zeros((ntile, 128, 16, TT), np.float32)
    for c in range(ncores):
        if c in REAL:
            s = REAL.index(c)
            xp_l[s] = np.ascontiguousarray(x_prompt[s].reshape(ntile, TT, 16, 128).transpose(0, 3, 2, 1))
        else:
            s = -1
            xp_l[s] = xzero
        bs = slice(c * NSMP, (c + 1) * NSMP)
        ck = cache_k[:, bs].reshape(L, NSMP, 128, 256)
        in_maps.append({
            "xp": xp_l[s],
            "xs": np.ascontiguousarray(x_sample[bs, 0, :].reshape(NSMP, 16, 128).transpose(2, 1, 0)),
            "sg": np.ascontiguousarray(state_gla[:, bs]),
            "sc": np.ascontiguousarray(state_conv[:, bs].reshape(L, NSMP, 2, 8, 128).transpose(0, 4, 3, 1, 2)),
            "ck": np.ascontiguousarray(ck),
            "ckT": np.ascontiguousarray(cache_k[:, bs].transpose(0, 4, 1, 3, 2)),
            "cv": np.ascontiguousarray(cache_v[:, bs].reshape(L, NSMP, 128, 256)),
            "w_in": w_in, "w_lr": w_lr, "w_branch": w_branch, "w_out": w_out, "w_up": w_up, "w_down": w_down,
            "prm": prm, "cst": cst,
        })
    res = run_bass_kernel_spmd(nc, in_maps, core_ids=list(range(ncores)))
    R = res.results
    y_prompt = np.stack([R[REAL[s]]["o_yp"].transpose(0, 3, 2, 1).reshape(SQ, D) for s in range(B)])
    y_sample = np.concatenate([R[c]["o_ys"].transpose(2, 1, 0).reshape(NSMP, 1, D) for c in range(ncores)])
    sgp = np.stack([R[REAL[s]]["o_sgp"] for s in range(B)], axis=1)
    sgs = np.concatenate([R[c]["o_sgs"] for c in range(ncores)], axis=1)
    scp = np.stack([R[REAL[s]]["o_scp"].transpose(0, 3, 2, 1).reshape(L, 2, 1024) for s in range(B)], axis=1)
    scs = np.concatenate([R[c]["o_scs"].transpose(0, 3, 4, 2, 1).reshape(L, NSMP, 2, 1024) for c in range(ncores)], axis=1)
    ckp = np.stack([R[REAL[s]]["o_ckp"].reshape(L, 128, 4, 64) for s in range(B)], axis=1)
    cks = np.concatenate([R[c]["o_cks"].reshape(L, NSMP, 128, 4, 64) for c in range(ncores)], axis=1)
    cvp = np.stack([R[REAL[s]]["o_cvp"].reshape(L, 128, 4, 64) for s in range(B)], axis=1)
    cvs = np.concatenate([R[c]["o_cvs"].reshape(L, NSMP, 128, 4, 64) for c in range(ncores)], axis=1)
    out = (y_prompt, y_sample, sgp, sgs, scp, scs, ckp, cks, cvp, cvs)
    return tuple(np.ascontiguousarray(o, dtype=np.float32) for o in out)
```
